# Optimizing a Trainium2 kernel written in Bass

```python
import math
import jax, jax.numpy as jnp
from jax import lax
import numpy as np

D_MODEL = 2048
BATCH = 2
SEQ = 8192
DEPTH = 1

NH_M = 4
DK_M = 128
DV_M = 256
CONV_K = 5
NH_H = 8
DK_H = 128
DV_H = 128
CHUNK = 64
N_EXPERTS = 16
CAP_FACTOR = 2
D_FF_EXPERT = 5632
LN_EPS = 1e-5
DEEPNORM_ALPHA = (2.0 * DEPTH) ** 0.25
DEEPNORM_BETA = (8.0 * DEPTH) ** -0.25

QK_M = NH_M * DK_M
V_M = NH_M * DV_M
Q_H = NH_H * DK_H
V_H = NH_H * DV_H
SPLIT_SIZES = (2 * QK_M, V_M, V_M, 2 * NH_M, 2 * NH_M, Q_H, 2 * Q_H, V_H, V_H, D_MODEL, D_MODEL)
D_IN = 2 * QK_M + 2 * V_M + 4 * NH_M + 3 * Q_H + 2 * V_H + 2 * D_MODEL

kernel_name = "bidir_mlstm_hgrn2_gated_ec_moe_deepnorm"


def layer_norm(x, g, b):
    xf = x.astype(jnp.float32)
    mu = jnp.mean(xf, axis=-1, keepdims=True)
    var = jnp.mean(jnp.square(xf - mu), axis=-1, keepdims=True)
    return ((xf - mu) * lax.rsqrt(var + LN_EPS) * g + b).astype(x.dtype)


def head_rms_norm(x, g):
    xf = x.astype(jnp.float32)
    return xf * lax.rsqrt(jnp.mean(xf * xf, axis=-1, keepdims=True) + LN_EPS) * g.astype(jnp.float32)


def centred_dwconv(x, w):
    C = x.shape[-1]
    pad = CONV_K // 2
    return lax.conv_general_dilated(x, w[:, None, :].astype(x.dtype), window_strides=(1,),
                                    padding=[(pad, pad)], dimension_numbers=("NWC", "WIO", "NWC"),
                                    feature_group_count=C)


def _to_chunks(t):
    N, S = t.shape[:2]
    t = t.reshape((N, S // CHUNK, CHUNK) + t.shape[2:])
    return jnp.moveaxis(jnp.moveaxis(t, 1, 0), 3, 2)


def _from_chunks(t):
    t = jnp.moveaxis(jnp.moveaxis(t, 2, 3), 0, 1)
    N, nc, L = t.shape[:3]
    return t.reshape((N, nc * L) + t.shape[3:])


def _bidirectional(fn, shared, gates):
    B = shared[0].shape[0]
    args = [jnp.concatenate([t, jnp.flip(t, axis=1)], axis=0) for t in shared]
    args += [jnp.concatenate([g[:, :, 0], jnp.flip(g[:, :, 1], axis=1)], axis=0) for g in gates]
    out = fn(*args)
    return out[:B] + jnp.flip(out[B:], axis=1)


def mlstm_chunkwise(q, k, v, log_i, log_f):
    N, S, H, dk = q.shape
    dv = v.shape[-1]
    f32 = jnp.float32
    qc, kc, vc = (_to_chunks(t.astype(f32)) for t in (q, k, v))
    lic, lfc = (_to_chunks(t.astype(f32)) for t in (log_i, log_f))
    tri = jnp.tril(jnp.ones((CHUNK, CHUNK), dtype=bool))

    def step(carry, inp):
        C, n, m = carry
        qb, kb, vb, li, lf = inp
        g = jnp.cumsum(lf, axis=-1)
        D = jnp.where(tri, g[..., :, None] - g[..., None, :] + li[..., None, :], -jnp.inf)
        inter = g + m[..., None]
        m_t = jnp.maximum(jnp.max(D, axis=-1), inter)
        W = jnp.exp(D - m_t[..., None]) * jnp.einsum("nhtd,nhsd->nhts", qb, kb)
        sc = jnp.exp(inter - m_t)
        num = jnp.einsum("nhts,nhsv->nhtv", W, vb) + sc[..., None] * jnp.einsum("nhtd,nhdv->nhtv", qb, C)
        den = jnp.sum(W, axis=-1) + sc * jnp.einsum("nhtd,nhd->nht", qb, n)
        h = num / jnp.maximum(jnp.abs(den), jnp.exp(-m_t))[..., None]
        m_new = m_t[..., -1]
        wk = jnp.exp(g[..., -1:] - g + li - m_new[..., None])
        decay = jnp.exp(g[..., -1] + m - m_new)
        C = decay[..., None, None] * C + jnp.einsum("nhs,nhsd,nhsv->nhdv", wk, kb, vb)
        n = decay[..., None] * n + jnp.einsum("nhs,nhsd->nhd", wk, kb)
        return (C, n, m_new), h

    init = (jnp.zeros((N, H, dk, dv), f32), jnp.zeros((N, H, dk), f32), jnp.zeros((N, H), f32))
    _, hs = lax.scan(step, init, (qc, kc, vc, lic, lfc))
    return _from_chunks(hs)


def hgrn2_chunkwise(q, i, f):
    N, S, H, dk = q.shape
    dv = i.shape[-1]
    f32 = jnp.float32
    qc, ic, fc = (_to_chunks(t.astype(f32)) for t in (q, i, f))
    tri = jnp.tril(jnp.ones((CHUNK, CHUNK), dtype=bool))[..., None]

    def step(state, inp):
        qb, ib, fb = inp
        kb = 1.0 - fb
        b = jnp.cumsum(jnp.log(fb), axis=-2)
        dec = jnp.exp(jnp.where(tri, b[..., :, None, :] - b[..., None, :, :], -jnp.inf))
        A = jnp.einsum("nhtd,nhsd,nhtsd->nhts", qb, kb, dec)
        o = jnp.einsum("nhts,nhsv->nhtv", A, ib) + jnp.einsum("nhtd,nhdv->nhtv", qb * jnp.exp(b), state)
        b_last = b[..., -1:, :]
        state = jnp.exp(b_last[..., 0, :])[..., None] * state + jnp.einsum(
            "nhsd,nhsv->nhdv", kb * jnp.exp(b_last - b), ib)
        return state, o

    _, os_ = lax.scan(step, jnp.zeros((N, H, dk, dv), f32), (qc, ic, fc))
    return _from_chunks(os_)


def hybrid_mixer(h, w_in, b_in, conv_w, mlstm_norm_g, hgrn_norm_g, lb, w_branch_m, w_branch_h, w_out):
    B, S, _ = h.shape
    f32 = jnp.float32
    proj = jnp.einsum("bsd,de->bse", h, w_in) + b_in
    cuts = [int(c) for c in np.cumsum(SPLIT_SIZES)[:-1]]
    (qk_m, v_m, o_m, i_m, f_m, q_h, f_h, i_h, g_h, gate_m, gate_h) = jnp.split(proj, cuts, axis=-1)

    qk = jax.nn.silu(centred_dwconv(qk_m, conv_w))
    q_m = qk[..., :QK_M].reshape(B, S, NH_M, DK_M)
    k_m = qk[..., QK_M:].reshape(B, S, NH_M, DK_M) * (DK_M ** -0.5)
    v = v_m.reshape(B, S, NH_M, DV_M)
    log_i = i_m.reshape(B, S, 2, NH_M).astype(f32)
    log_f = jax.nn.log_sigmoid(f_m.reshape(B, S, 2, NH_M).astype(f32))
    hm = _bidirectional(mlstm_chunkwise, (q_m, k_m, v), (log_i, log_f))
    hm = head_rms_norm(hm, mlstm_norm_g) * jax.nn.sigmoid(o_m.reshape(B, S, NH_M, DV_M).astype(f32))
    y_m = jnp.einsum("bshv,hvd->bsd", hm.astype(h.dtype), w_branch_m.reshape(NH_M, DV_M, D_MODEL))

    lbf = lb.reshape(2, NH_H, DK_H)
    f = lbf + (1.0 - lbf) * jax.nn.sigmoid(f_h.reshape(B, S, 2, NH_H, DK_H).astype(f32))
    ho = _bidirectional(hgrn2_chunkwise,
                        (q_h.reshape(B, S, NH_H, DK_H), i_h.reshape(B, S, NH_H, DV_H)), (f,))
    ho = head_rms_norm(ho, hgrn_norm_g) * jax.nn.silu(g_h.reshape(B, S, NH_H, DV_H).astype(f32))
    y_h = jnp.einsum("bshv,hvd->bsd", ho.astype(h.dtype), w_branch_h.reshape(NH_H, DV_H, D_MODEL))

    merged = jax.nn.sigmoid(gate_m) * y_m + jax.nn.sigmoid(gate_h) * y_h
    return jnp.einsum("bsd,de->bse", merged, w_out)


def expert_choice_ffn(x, w_router, w_gate, w_up, w_down):
    B, S, D = x.shape
    cap = CAP_FACTOR * S // N_EXPERTS
    logits = jnp.einsum("bsd,de->bse", x, w_router).astype(jnp.float32)
    aff = jax.nn.softmax(logits, axis=-1)
    gate, idx = lax.top_k(jnp.swapaxes(aff, 1, 2), cap)
    bidx = jnp.arange(B)[:, None, None]
    xe = x[bidx, idx]
    hid = jax.nn.silu(jnp.einsum("becd,edf->becf", xe, w_gate)) * jnp.einsum("becd,edf->becf", xe, w_up)
    ye = jnp.einsum("becf,efd->becd", hid, w_down) * gate[..., None].astype(x.dtype)
    return jnp.zeros_like(x).at[bidx, idx].add(ye)


def setup_inputs(seed: int = 0) -> dict:
    key = jax.random.key(seed)
    ks = jax.random.split(key, 24)
    nrm = jax.random.normal
    f32 = jnp.float32
    x = nrm(ks[0], (BATCH, SEQ, D_MODEL), f32)
    ln_in_g = 1.0 + 0.02 * nrm(ks[1], (D_MODEL,), f32)
    ln_in_b = 0.02 * nrm(ks[2], (D_MODEL,), f32)
    hgrn_lb_logits = 0.5 * nrm(ks[3], (2, DEPTH + 1, Q_H), f32)
    w_in = nrm(ks[4], (DEPTH, D_MODEL, D_IN), f32) * D_MODEL ** -0.5
    f_off = 2 * QK_M + 2 * V_M + 2 * NH_M
    b_in = 0.02 * nrm(ks[5], (DEPTH, D_IN), f32)
    b_in = b_in.at[:, f_off:f_off + 2 * NH_M].add(jnp.tile(jnp.linspace(3.0, 6.0, NH_M), 2))
    conv_w = nrm(ks[6], (DEPTH, CONV_K, 2 * QK_M), f32) * CONV_K ** -0.5
    mlstm_norm_g = 1.0 + 0.02 * nrm(ks[7], (DEPTH, NH_M, DV_M), f32)
    hgrn_norm_g = 1.0 + 0.02 * nrm(ks[8], (DEPTH, NH_H, DV_H), f32)
    w_branch_m = nrm(ks[9], (DEPTH, V_M, D_MODEL), f32) * (V_M ** -0.5) * DEEPNORM_BETA
    w_branch_h = nrm(ks[10], (DEPTH, V_H, D_MODEL), f32) * (V_H ** -0.5) * DEEPNORM_BETA
    w_out = nrm(ks[11], (DEPTH, D_MODEL, D_MODEL), f32) * (D_MODEL ** -0.5) * DEEPNORM_BETA
    ln1_g = 1.0 + 0.02 * nrm(ks[12], (DEPTH, D_MODEL), f32)
    ln1_b = 0.02 * nrm(ks[13], (DEPTH, D_MODEL), f32)
    w_router = nrm(ks[14], (DEPTH, D_MODEL, N_EXPERTS), f32) * D_MODEL ** -0.5
    w_gate_e = nrm(ks[15], (DEPTH, N_EXPERTS, D_MODEL, D_FF_EXPERT), f32) * D_MODEL ** -0.5
    w_up_e = nrm(ks[16], (DEPTH, N_EXPERTS, D_MODEL, D_FF_EXPERT), f32) * D_MODEL ** -0.5
    w_down_e = nrm(ks[17], (DEPTH, N_EXPERTS, D_FF_EXPERT, D_MODEL), f32) * (D_FF_EXPERT ** -0.5) * DEEPNORM_BETA
    ln2_g = 1.0 + 0.02 * nrm(ks[18], (DEPTH, D_MODEL), f32)
    ln2_b = 0.02 * nrm(ks[19], (DEPTH, D_MODEL), f32)
    return {"x": x, "ln_in_g": ln_in_g, "ln_in_b": ln_in_b, "hgrn_lb_logits": hgrn_lb_logits,
            "w_in": w_in, "b_in": b_in, "conv_w": conv_w, "mlstm_norm_g": mlstm_norm_g,
            "hgrn_norm_g": hgrn_norm_g, "w_branch_m": w_branch_m, "w_branch_h": w_branch_h,
            "w_out": w_out, "ln1_g": ln1_g, "ln1_b": ln1_b, "w_router": w_router,
            "w_gate_e": w_gate_e, "w_up_e": w_up_e, "w_down_e": w_down_e, "ln2_g": ln2_g, "ln2_b": ln2_b}


def reference(x, ln_in_g, ln_in_b, hgrn_lb_logits, w_in, b_in, conv_w, mlstm_norm_g, hgrn_norm_g,
              w_branch_m, w_branch_h, w_out, ln1_g, ln1_b, w_router, w_gate_e, w_up_e, w_down_e,
              ln2_g, ln2_b):
    h = layer_norm(x, ln_in_g, ln_in_b)
    lb_all = jnp.cumsum(jax.nn.softmax(hgrn_lb_logits.astype(jnp.float32), axis=1), axis=1)
    for l in range(DEPTH):
        mix = hybrid_mixer(h, w_in[l], b_in[l], conv_w[l], mlstm_norm_g[l], hgrn_norm_g[l], lb_all[:, l],
                           w_branch_m[l], w_branch_h[l], w_out[l])
        h = layer_norm(DEEPNORM_ALPHA * h + mix, ln1_g[l], ln1_b[l])
        ffn = expert_choice_ffn(h, w_router[l], w_gate_e[l], w_up_e[l], w_down_e[l])
        h = layer_norm(DEEPNORM_ALPHA * h + ffn, ln2_g[l], ln2_b[l])
    return h
```

```python
import numpy as np
from contextlib import ExitStack
import concourse.bass as bass
import concourse.mybir as mybir
from concourse.bass_utils import run_bass_kernel_spmd

F32 = mybir.dt.float32
BF16 = mybir.dt.bfloat16
I32 = mybir.dt.int32
ALU = mybir.AluOpType
AF = mybir.ActivationFunctionType
AX = mybir.AxisListType

SEM_LIMIT = 30000


class _Op:
    __slots__ = ("eng", "fn", "deps", "kind", "sig", "idx", "skip_pe")

    def __init__(self, eng, fn, kind):
        self.eng = eng
        self.fn = fn
        self.kind = kind
        self.deps = []
        self.sig = None
        self.idx = -1


class FW:
    ENG = ("pe", "act", "dve", "pool", "sp")

    def __init__(self, nc):
        self.nc = nc
        self.ops = []
        self.track = {}
        self.semctx = []
        self.cur = {}
        self.waited = {}
        self.all_sems = []
        self.last_sig = {}
        self.dq = {}
        self.prewait = {}

    def _newsem(self, name):
        self.nsem = getattr(self, "nsem", 0) + 1
        name = "%s_u%d" % (name, self.nsem)
        cm = self.nc.semaphore(name)
        s = cm.__enter__()
        self.semctx.append(cm)
        return s

    def close(self):
        for cm in reversed(self.semctx):
            cm.__exit__(None, None, None)
        self.semctx = []

    def op(self, eng, fn, reads=(), writes=(), kind="c"):
        o = _Op(eng, fn, kind)
        o.idx = len(self.ops)
        deps = set()
        for k in reads:
            t = self.track.get(k)
            if t is None:
                t = [None, []]
                self.track[k] = t
            if t[0] is not None:
                deps.add(t[0])
            t[1].append(o)
        for k in writes:
            t = self.track.get(k)
            if t is None:
                t = [None, []]
                self.track[k] = t
            if t[0] is not None:
                deps.add(t[0])
            for r in t[1]:
                if r is not o:
                    deps.add(r)
            t[0] = o
            t[1] = []
        deps.discard(o)
        o.deps = [d for d in deps]
        self.ops.append(o)
        return o

    def dma(self, out, in_, reads, writes, eng="sp", **kw):
        return self.op(eng, lambda e: e.dma_start(out=out, in_=in_, **kw), reads, writes, kind="d")

    def emit(self, barrier_first=True):
        nc = self.nc
        ops = self.ops
        prev_barrier = dict(self.last_sig) if barrier_first else {}
        src = set()
        for o in ops:
            for d in o.deps:
                if d.sig is None:
                    if d.eng == "pe" and o.eng == "pe" and d.kind == "c" and o.kind == "c":
                        continue
                    src.add(d)
        last = {}
        for o in ops:
            last[(o.eng, o.kind)] = o
        for o in last.values():
            src.add(o)
        for o in ops:
            if o.kind != "c":
                src.add(o)
        K = 16
        for o in ops:
            if o in src:
                key = (o.eng, o.kind)
                if o.kind == "cc":
                    s = self._newsem("cc%d" % o.idx)
                    o.sig = (s, 1)
                    continue
                if o.kind == "d":
                    q = self.dq.setdefault(o.eng, {"n": 0, "slots": [None] * K})
                    j = q["n"] % K
                    q["n"] += 1
                    c = q["slots"][j]
                    pre = None
                    if c is not None and c[1] > 0:
                        pre = (c[0], c[1])
                    if c is None or c[1] + 16 > SEM_LIMIT:
                        c = [self._newsem("d_%s_%d_%d" % (o.eng, j, o.idx)), 0]
                        q["slots"][j] = c
                    c[1] += 16
                    o.sig = (c[0], c[1])
                    self.prewait[o] = pre
                    self.last_sig[(o.eng, "d", j)] = o.sig
                    continue
                c = self.cur.get(key)
                if c is None or c[1] + 1 > SEM_LIMIT:
                    c = [self._newsem("s_%s_%s_%d" % (o.eng, o.kind, o.idx)), 0]
                    self.cur[key] = c
                c[1] += 1
                o.sig = (c[0], c[1])
        for k, o in last.items():
            if o.kind != "d":
                self.last_sig[k] = o.sig
        engmap = {"pe": "tensor", "act": "scalar", "dve": "vector", "pool": "gpsimd", "sp": "sync"}
        by_eng = {e: [o for o in ops if o.eng == e] for e in self.ENG}
        waited = self.waited

        def run_engine(ename, e):
            first = True
            for o in by_eng[ename]:
                need = {}
                if first:
                    first = False
                    for bk, sg in prev_barrier.items():
                        if sg is not None:
                            cur = need.get(id(sg[0]))
                            if cur is None or cur[1] < sg[1]:
                                need[id(sg[0])] = (sg[0], sg[1])
                pre = self.prewait.pop(o, None)
                if pre is not None:
                    cur = need.get(id(pre[0]))
                    if cur is None or cur[1] < pre[1]:
                        need[id(pre[0])] = pre
                for d in o.deps:
                    if d.sig is None:
                        continue
                    if d.eng == "pe" and o.eng == "pe" and d.kind == "c" and o.kind == "c":
                        continue
                    s, v = d.sig
                    cur = need.get(id(s))
                    if cur is None or cur[1] < v:
                        need[id(s)] = (s, v)
                for sid, (s, v) in need.items():
                    wk = (ename, sid)
                    if waited.get(wk, 0) >= v:
                        continue
                    waited[wk] = v
                    e.wait_ge(s, v)
                ins = o.fn(e)
                if o.sig is not None:
                    if o.kind == "d":
                        ins.then_inc(o.sig[0], 16)
                    elif o.kind == "cc":
                        ins.then_inc(o.sig[0])
                    else:
                        ins.then_inc(o.sig[0], 1)

        with nc.Block() as block:
            @block.tensor
            def _(e):
                run_engine("pe", e)

            @block.scalar
            def _(e):
                run_engine("act", e)

            @block.vector
            def _(e):
                run_engine("dve", e)

            @block.gpsimd
            def _(e):
                run_engine("pool", e)

            @block.sync
            def _(e):
                run_engine("sp", e)
        self.ops = []

    def final_wait(self, eng_name="pool"):
        nc = self.nc
        sigs = [sg for sg in self.last_sig.values() if sg is not None]
        with nc.Block() as block:
            @block.gpsimd
            def _(e):
                for s, v in sigs:
                    e.wait_ge(s, v)


S = 8192
D = 2048
NT = S // 128
EPS = 1e-5
ALPHA = 2.0 ** 0.25


def host_consts():
    s = np.arange(128)[:, None]
    t = np.arange(128)[None, :]
    c = {}
    c["ident"] = np.eye(128, dtype=np.float32)
    c["trif"] = (s <= t).astype(np.float32)
    c["trib"] = (s >= t).astype(np.float32)
    c["ones"] = np.ones((128, 128), np.float32)
    same = (s // 64) == (t // 64)
    c["hmaskf"] = (same & (s <= t)).astype(np.float32)
    c["hmaskb"] = (same & (s >= t)).astype(np.float32)
    midf = (t // 64) * 64 + 31
    midb = (t // 64) * 64 + 32
    c["Mf"] = (same * ((s <= t).astype(np.float32) - (s <= midf).astype(np.float32))).astype(np.float32)
    c["Mb"] = (same * ((s >= t).astype(np.float32) - (s >= midb).astype(np.float32))).astype(np.float32)
    xf = np.zeros((128, 6), np.float32)
    xb = np.zeros((128, 6), np.float32)
    sv = np.arange(128)
    for ch in range(2):
        inch = (sv // 64) == ch
        mf = ch * 64 + 31
        mb = ch * 64 + 32
        xf[:, 3 * ch + 0] = inch & (sv <= mf)
        xf[:, 3 * ch + 1] = inch
        xf[:, 3 * ch + 2] = inch & (sv > mf)
        xb[:, 3 * ch + 0] = inch & (sv >= mb)
        xb[:, 3 * ch + 1] = inch
        xb[:, 3 * ch + 2] = inch & (sv < mb)
    c["Mxf"] = np.concatenate([c["Mf"], xf], 1)
    c["Mxb"] = np.concatenate([c["Mb"], xb], 1)
    return c


CONST_SHAPES = {"ident": [128, 128], "trif": [128, 128], "trib": [128, 128], "ones": [128, 128],
                "hmaskf": [128, 128], "hmaskb": [128, 128], "Mf": [128, 128], "Mb": [128, 128],
                "Mxf": [128, 134], "Mxb": [128, 134]}


def ln_rows(fw, T, x_t, key, g_t, b_t, out_t, okey, tmp):
    sq, ssum, ssq, mean, var, rstd, nb = tmp
    k = key
    fw.op("dve", lambda e: e.reduce_sum(ssum[:], x_t, AX.X), [k], ["ln_ssum"])
    fw.op("act", lambda e: e.activation(sq[:], x_t, AF.Square), [k], ["ln_sq"])
    fw.op("dve", lambda e: e.reduce_sum(ssq[:], sq[:], AX.X), ["ln_sq"], ["ln_ssq"])
    fw.op("dve", lambda e: e.tensor_scalar(mean[:], ssum[:], 1.0 / D, None, ALU.mult), ["ln_ssum"], ["ln_mean"])
    fw.op("dve", lambda e: e.tensor_tensor(var[:], mean[:], mean[:], ALU.mult), ["ln_mean"], ["ln_var"])
    fw.op("dve", lambda e: e.scalar_tensor_tensor(var[:], ssq[:], 1.0 / D, var[:], ALU.mult, ALU.subtract), ["ln_ssq", "ln_var"], ["ln_var"])
    fw.op("dve", lambda e: e.tensor_scalar(var[:], var[:], EPS, None, ALU.add), ["ln_var"], ["ln_var"])
    fw.op("act", lambda e: e.activation(rstd[:], var[:], AF.Sqrt), ["ln_var"], ["ln_rstd"])
    fw.op("dve", lambda e: e.reciprocal(rstd[:], rstd[:]), ["ln_rstd"], ["ln_rstd"])
    fw.op("dve", lambda e: e.scalar_tensor_tensor(nb[:], mean[:], -1.0, rstd[:], ALU.mult, ALU.mult), ["ln_mean", "ln_rstd"], ["ln_nb"])
    fw.op("act", lambda e: e.activation(sq[:], x_t, AF.Identity, bias=nb[:], scale=rstd[:]), [k, "ln_nb", "ln_rstd", "ln_sq"], ["ln_sq"])
    fw.op("pool", lambda e: e.tensor_tensor(sq[:], sq[:], g_t, ALU.mult), ["ln_sq", "lng"], ["ln_sq"])
    fw.op("dve", lambda e: e.tensor_tensor(out_t, sq[:], b_t, ALU.add), ["ln_sq", "lnb"], [okey])


def build_phase_a(nc, fw, io, dbg=False):
    xb = io["xb"]
    d_fm = nc.dram_tensor("d_fm", [512, S + 4], F32)
    d_tm = nc.dram_tensor("d_tm", [S, 1540], F32)
    d_hm = nc.dram_tensor("d_hm", [S, 256], F32)
    d_ho = nc.dram_tensor("d_ho", [S, 256], F32)
    hg_out = io["hg_out"]
    C = io["consts"]

    with ExitStack() as st:
        T = lambda n, sh, dt: st.enter_context(nc.sbuf_tensor("s_" + n, sh, dt))
        P = lambda n, sh, dt: st.enter_context(nc.psum_tensor("p_" + n, sh, dt))
        wfm = T("wfm", [128, 16, 512], BF16)
        wtm = T("wtm", [128, 16, 1540], BF16)
        lng = T("lng", [128, D], F32)
        lnb = T("lnb", [128, D], F32)
        bfm = T("bfm", [128, 4, 1], F32)
        btm = T("btm", [128, 1540], F32)
        identb = T("identb", [128, 128], BF16)
        zero = T("zero", [128, 4, 2], F32)
        xt = [T("xt%d" % i, [128, D], F32) for i in range(2)]
        sq = T("sq", [128, D], F32)
        hb = [T("hb%d" % i, [128, D], BF16) for i in range(2)]
        hT = [T("hT%d" % i, [128, 16, 128], BF16) for i in range(2)]
        fmo = [T("fmo%d" % i, [128, 4, 128], F32) for i in range(2)]
        tmo = [T("tmo%d" % i, [128, 1540], F32) for i in range(2)]
        small = [T("sm%d" % i, [128, 1], F32) for i in range(6)]
        pT = [P("pT%d" % i, [128, 8, 128], BF16) for i in range(2)]
        pfm = P("pfm", [128, 4, 128], F32)
        ptm = [P("ptm%d" % i, [128, 512], F32) for i in range(3)]
        pg = P("pg", [128, 4], F32)
        for k in range(16):
            fw.dma(wfm[:, k, :], io["wfm"][k * 128:(k + 1) * 128, :], [], [("wfm", k)], eng="pool")
            fw.dma(wtm[:, k, :], io["wtm"][k * 128:(k + 1) * 128, :], [], [("wtm", k)], eng="pool")
        fw.dma(lng[:], io["ln_g"], [], ["lng"])
        fw.dma(lnb[:], io["ln_b"], [], ["lnb"])
        fw.dma(bfm[:], io["bfm"], [], ["bfm"])
        fw.dma(btm[:], io["btm"], [], ["btm"])
        fw.dma(identb[:], C["ident"], [], ["identb"], eng="pool")
        fw.op("dve", lambda e: e.memset(zero[:], 0.0), [], ["zero"])
        fw.dma(d_fm[:, 0:2].rearrange("(b p) t -> p b t", p=128), zero[:], ["zero"], ["d_fm_h0"])
        fw.dma(d_fm[:, S + 2:S + 4].rearrange("(b p) t -> p b t", p=128), zero[:], ["zero"], ["d_fm_h1"])
        def stage_ln(n):
            x_ = xt[n % 2]
            fw.dma(x_[:], xb[n * 128:(n + 1) * 128, :], [], [("xt", n % 2)])
            ln_rows(fw, T, x_[:], ("xt", n % 2), lng[:], lnb[:], hb[n % 2][:], ("hb", n % 2), (sq,) + tuple(small))

        def stage_mm(n):
            hT_ = hT[n % 2]
            hb_ = hb[n % 2]
            for half in range(2):
                pt = pT[half]
                for j in range(8):
                    k = half * 8 + j
                    fw.op("pe", lambda e, pt=pt, j=j, k=k: e.transpose(pt[:, j, :], hb_[:, k * 128:(k + 1) * 128], identb[:]), [("hb", n % 2), "identb"], [("pT", half)])
                if half == 0:
                    fw.op("act", lambda e, pt=pt, hT_=hT_: e.copy(hT_[:, 0:8, :], pt[:]), [("pT", 0)], [("hT", n % 2, 0)])
                else:
                    fw.op("dve", lambda e, pt=pt, hT_=hT_: e.tensor_copy(hT_[:, 8:16, :], pt[:]), [("pT", 1)], [("hT", n % 2, 1)])
            hk = [("hT", n % 2, 0), ("hT", n % 2, 1)]
            for blk in range(4):
                for k in range(16):
                    fw.op("pe", lambda e, blk=blk, k=k, hT_=hT_: e.matmul(pfm[:, blk, :], wfm[:, k, blk * 128:(blk + 1) * 128], hT_[:, k, :], start=(k == 0), stop=(k == 15)),
                          hk + [("wfm", k)], ["pfm"])
            fo = fmo[n % 2]
            fw.op("dve", lambda e, fo=fo: e.tensor_tensor(fo[:], pfm[:], bfm[:].to_broadcast([128, 4, 128]), ALU.add), ["pfm", "bfm"], [("fmo", n % 2)])
            fw.dma(d_fm[:, 2 + n * 128:2 + (n + 1) * 128].rearrange("(b p) t -> p b t", p=128), fo[:], [("fmo", n % 2)], [("d_fm", n)], eng="act")
            to = tmo[n % 2]
            for cb in range(3):
                for k in range(16):
                    fw.op("pe", lambda e, cb=cb, k=k, hT_=hT_: e.matmul(ptm[cb][:], hT_[:, k, :], wtm[:, k, cb * 512:(cb + 1) * 512], start=(k == 0), stop=(k == 15)),
                          hk + [("wtm", k)], [("ptm", cb)])
                eng = "dve" if cb != 1 else "pool"
                if eng == "pool":
                    fw.op("act", lambda e, cb=cb, to=to: e.copy(to[:, cb * 512:(cb + 1) * 512], ptm[cb][:]), [("ptm", cb)], [("tmo", n % 2, cb)])
                    fw.op("pool", lambda e, cb=cb, to=to: e.tensor_tensor(to[:, cb * 512:(cb + 1) * 512], to[:, cb * 512:(cb + 1) * 512], btm[:, cb * 512:(cb + 1) * 512], ALU.add), [("tmo", n % 2, cb), "btm"], [("tmo", n % 2, cb)])
                else:
                    fw.op("dve", lambda e, cb=cb, to=to: e.tensor_tensor(to[:, cb * 512:(cb + 1) * 512], ptm[cb][:], btm[:, cb * 512:(cb + 1) * 512], ALU.add), [("ptm", cb), "btm"], [("tmo", n % 2, cb)])
            for k in range(16):
                fw.op("pe", lambda e, k=k, hT_=hT_: e.matmul(pg[:], hT_[:, k, :], wtm[:, k, 1536:1540], start=(k == 0), stop=(k == 15)), hk + [("wtm", k)], ["pg"])
            fw.op("dve", lambda e, to=to: e.tensor_tensor(to[:, 1536:1540], pg[:], btm[:, 1536:1540], ALU.add), ["pg", "btm"], [("tmo", n % 2, 3)])
            fw.dma(d_tm[n * 128:(n + 1) * 128, :], to[:], [("tmo", n % 2, i) for i in range(4)], [("d_tm", n)], eng="act")

        stage_ln(0)
        for n in range(NT):
            if n + 1 < NT:
                stage_ln(n + 1)
            stage_mm(n)
        fw.emit()

    with ExitStack() as st:
        T = lambda n, sh, dt: st.enter_context(nc.sbuf_tensor("s_" + n, sh, dt))
        P = lambda n, sh, dt: st.enter_context(nc.psum_tensor("p_" + n, sh, dt))
        cn = {}
        for name in ["ident", "trif", "trib", "ones", "hmaskf", "hmaskb", "Mf", "Mb", "Mxf", "Mxb"]:
            cn[name] = T("c_" + name, CONST_SHAPES[name], F32)
            fw.dma(cn[name][:], C[name], [], ["c_" + name])
        identb = T("identb2", [128, 128], BF16)
        fw.dma(identb[:], C["ident"], [], ["identb"], eng="pool")
        cw = T("cw", [128, 2, 5], F32)
        fw.dma(cw[:], io["cw"], [], ["cw"])
        gnm = T("gnm", [128, 256], F32)
        gnh = T("gnh", [128, 256], F32)
        fw.dma(gnm[:], io["gn_m"], [], ["gnm"])
        fw.dma(gnh[:], io["gn_h"], [], ["gnh"])
        lb = T("lb", [128, 512], F32)
        oml = T("oml", [128, 512], F32)
        l1 = T("l1", [128, 512], F32)
        fw.dma(lb[:], io["lb0"], [], ["lb"])
        fw.dma(l1[:], io["lb1"], [], ["l1"])
        fw.op("dve", lambda e: e.tensor_tensor(lb[:], lb[:], l1[:], ALU.subtract), ["lb", "l1"], ["lb"])
        fw.op("act", lambda e: e.activation(lb[:], lb[:], AF.Sigmoid), ["lb"], ["lb"])
        fw.op("dve", lambda e: e.tensor_scalar(oml[:], lb[:], -1.0, 1.0, ALU.mult, ALU.add), ["lb"], ["oml"])

        gates = T("gates", [128, NT, 4], F32)
        for n0 in range(0, NT, 8):
            fw.dma(gates[:, n0:n0 + 8, :], d_tm[n0 * 128:(n0 + 8) * 128, 1536:1540].rearrange("(n p) c -> p n c", p=128), [("d_tm", n) for n in range(n0, n0 + 8)], ["gates"])
        lf = T("lf", [128, 2, NT], F32)
        li = T("li", [128, 2, NT], F32)
        zz = T("zz", [128, 2, NT], F32)
        egl = T("egl", [128, 2, NT], F32)
        Zs = T("Zs", [128, 2, NT], F32)
        Ee = T("Ee", [128, 2, NT], F32)
        aa = T("aa", [128, 2, NT], F32)
        sc = T("sc", [128, 2, NT], F32)
        thr = T("thr", [128, 2, NT], F32)
        gcs = T("gcs", [128, 2, NT], F32)
        pst = ExitStack()
        PP = lambda n, sh, dt: pst.enter_context(nc.psum_tensor("pp_" + n, sh, dt))
        pA = PP("pA", [128, 2, NT], F32)
        pB = PP("pB", [128, 2, NT], F32)
        for d in range(2):
            fw.op("act", lambda e, d=d: e.activation(lf[:, d, :], gates[:, :, 2 + d], AF.Exp, scale=-1.0), ["gates"], [("lf", d)])
            fw.op("dve", lambda e, d=d: e.tensor_copy(li[:, d, :], gates[:, :, d]), ["gates"], [("li", d)])
        for d in range(2):
            fw.op("act", lambda e, d=d: e.activation(lf[:, d, :], lf[:, d, :], AF.Ln, bias=1.0), [("lf", d)], [("lf", d)])
            fw.op("dve", lambda e, d=d: e.tensor_scalar(lf[:, d, :], lf[:, d, :], -1.0, None, ALU.mult), [("lf", d)], [("lf", d)])
        tri = [cn["trif"], cn["trib"]]
        for d in range(2):
            fw.op("pe", lambda e, d=d: e.matmul(pA[:, d, :], tri[d][:], lf[:, d, :], start=True, stop=True), [("lf", d), "c_trif", "c_trib"], [("pA", d)])
            fw.op("pe", lambda e, d=d: e.matmul(pB[:, d, :], cn["ones"][:], lf[:, d, :], start=True, stop=True), [("lf", d), "c_ones"], [("pB", d)])
        fw.op("dve", lambda e: e.tensor_copy(gcs[:], pA[:]), [("pA", 0), ("pA", 1)], ["gcs"])
        fw.op("act", lambda e: e.activation(egl[:], pB[:], AF.Exp), [("pB", 0), ("pB", 1)], ["egl"])
        fw.op("dve", lambda e: e.tensor_tensor(zz[:], li[:], gcs[:], ALU.subtract), [("li", 0), ("li", 1), "gcs"], ["zz"])
        fw.op("act", lambda e: e.activation(zz[:], zz[:], AF.Exp), ["zz"], ["zz"])
        fw.op("act", lambda e: e.activation(thr[:], gcs[:], AF.Exp, scale=-1.0), ["gcs"], ["thr"])
        for d in range(2):
            fw.op("pe", lambda e, d=d: e.matmul(pA[:, d, :], cn["ones"][:], zz[:, d, :], start=True, stop=True), ["zz", "c_ones", "gcs"], [("pA", d)])
        fw.op("dve", lambda e: e.tensor_copy(Zs[:], pA[:]), [("pA", 0), ("pA", 1)], ["Zs"])
        fw.op("dve", lambda e: e.memset(Ee[:, 0, 0:1], 1.0), [], ["Ee"])
        fw.op("dve", lambda e: e.memset(Ee[:, 1, NT - 1:NT], 1.0), ["Ee"], ["Ee"])
        for n in range(NT - 1):
            fw.op("dve", lambda e, n=n: e.scalar_tensor_tensor(Ee[:, 0, n + 1:n + 2], Ee[:, 0, n:n + 1], Zs[:, 0, n:n + 1], egl[:, 0, n:n + 1], ALU.add, ALU.mult), ["Ee", "Zs", "egl"], ["Ee"])
            m = NT - 1 - n
            fw.op("dve", lambda e, m=m: e.scalar_tensor_tensor(Ee[:, 1, m - 1:m], Ee[:, 1, m:m + 1], Zs[:, 1, m:m + 1], egl[:, 1, m:m + 1], ALU.add, ALU.mult), ["Ee", "Zs", "egl"], ["Ee"])
        fw.op("dve", lambda e: e.tensor_tensor(Zs[:], Zs[:], Ee[:], ALU.add), ["Zs", "Ee"], ["Zs"])
        fw.op("dve", lambda e: e.reciprocal(Zs[:], Zs[:]), ["Zs"], ["Zs"])
        fw.op("dve", lambda e: e.tensor_tensor(sc[:], Ee[:], Zs[:], ALU.mult), ["Zs", "Ee"], ["sc"])
        fw.op("dve", lambda e: e.tensor_tensor(thr[:], thr[:], Zs[:], ALU.mult), ["Zs", "thr"], ["thr"])
        fw.op("dve", lambda e: e.scalar_tensor_tensor(aa[:], zz[:], 128.0 ** -0.5, Zs[:], ALU.mult, ALU.mult), ["Zs", "zz"], ["aa"])

        qT = T("qT", [128, NT, 128], BF16)
        kT = T("kT", [128, NT, 128], BF16)
        ktm = T("ktm", [128, NT, 128], BF16)
        vall = T("vall", [128, NT, 257], BF16)
        fw.op("pool", lambda e: e.memset(vall[:, :, 256:257], 1.0), [], ["vones"])
        for n0 in range(0, NT, 8):
            fw.dma(vall[:, n0:n0 + 8, 0:256], d_tm[n0 * 128:(n0 + 8) * 128, 0:256].rearrange("(n p) c -> p n c", p=128),
                   [("d_tm", n) for n in range(n0, n0 + 8)], [("vall", n0)], eng="pool")
        cin = [T("cin%d" % i, [128, 2, 132], F32) for i in range(2)]
        cacc = [T("cacc%d" % i, [128, 2, 128], F32) for i in range(2)]
        ctmp = T("ctmp", [128, 128], F32)
        pk = [PP("pk%d" % i, [128, 128], BF16) for i in range(2)]
        for n in range(NT):
            ci = cin[n % 2]
            ca = cacc[n % 2]
            fw.dma(ci[:], d_fm[0:256, n * 128:n * 128 + 132].rearrange("(b p) t -> p b t", p=128),
                   [("d_fm", m) for m in range(max(0, n - 1), min(NT, n + 2))] + ["d_fm_h0", "d_fm_h1"], [("cin", n % 2)])
            for qk, eng in ((0, "dve"), (1, "dve")):
                fw.op(eng, lambda e, qk=qk, ci=ci, ca=ca: e.tensor_scalar(ca[:, qk, :], ci[:, qk, 0:128], cw[:, qk, 0:1], None, ALU.mult), [("cin", n % 2), "cw"], [("cacc", n % 2, qk)])
                for j in range(1, 5):
                    if eng == "dve":
                        fw.op(eng, lambda e, qk=qk, ci=ci, ca=ca, j=j: e.scalar_tensor_tensor(ca[:, qk, :], ci[:, qk, j:j + 128], cw[:, qk, j:j + 1], ca[:, qk, :], ALU.mult, ALU.add),
                              [("cin", n % 2), "cw", ("cacc", n % 2, qk)], [("cacc", n % 2, qk)])
                    else:
                        fw.op(eng, lambda e, qk=qk, ci=ci, j=j: e.tensor_scalar(ctmp[:], ci[:, qk, j:j + 128], cw[:, qk, j:j + 1], None, ALU.mult), [("cin", n % 2), "cw"], ["ctmp"])
                        fw.op(eng, lambda e, qk=qk, ca=ca: e.tensor_tensor(ca[:, qk, :], ca[:, qk, :], ctmp[:], ALU.add), ["ctmp", ("cacc", n % 2, qk)], [("cacc", n % 2, qk)])
            fw.op("act", lambda e, ca=ca, n=n: e.activation(qT[:, n, :], ca[:, 0, :], AF.Silu), [("cacc", n % 2, 0)], [("qT", n)])
            fw.op("act", lambda e, ca=ca, n=n: e.activation(kT[:, n, :], ca[:, 1, :], AF.Silu), [("cacc", n % 2, 1)], [("kT", n)])
            fw.op("pe", lambda e, n=n: e.transpose(pk[n % 2][:], kT[:, n, :], identb[:]), [("kT", n), "identb"], [("pk", n % 2)])
            fw.op("act", lambda e, n=n: e.copy(ktm[:, n, :], pk[n % 2][:]), [("pk", n % 2)], [("ktm", n)])
        fw.emit()
        pst.close()

        Cst = [T("Cst%d" % d, [128, 257], F32) for d in range(2)]
        Sst = [[T("Sst%d_%d" % (d, h), [128, 128], F32) for h in range(2)] for d in range(2)]
        R2 = lambda name, sh, dt, cnt=2: [T("%s%d" % (name, i), sh, dt) for i in range(cnt)]
        WT = R2("WT", [128, 128], BF16)
        kp = R2("kp", [128, 128], BF16)
        Csc32 = R2("Csc32", [128, 257], F32)
        Csc16 = R2("Csc16", [128, 257], BF16)
        den = R2("den", [128, 1], F32)
        hmo = R2("hmo", [128, 256], F32)
        hfw = R2("hfw", [128, 256], F32)
        osig = R2("osig", [128, 256], F32)
        hsq = R2("hsq", [128, 256], F32)
        ssq = R2("ssq", [128, 1], F32)
        hgo = R2("hgo", [128, 256], BF16)
        fpre = R2("fpre", [128, 256], F32)
        ff = R2("ff", [128, 256], F32)
        logf = R2("logf", [128, 256], F32)
        kkk = R2("kkk", [128, 256], F32)
        eneg = R2("eneg", [128, 256], F32)
        ktl = R2("ktl", [128, 256], BF16)
        qin = R2("qin", [128, 2, 128], F32)
        itl = R2("itl", [128, 256], BF16)
        Eq = R2("Eq", [128, 128], F32, 4)
        ex = R2("ex", [128, 6], F32, 4)
        Qz = [T("Qz%d" % i, [128, 2, 128], BF16) for i in range(4)]
        ktT = R2("ktT", [128, 128], BF16, 4)
        AT = R2("AT", [128, 128], BF16, 4)
        Smid = R2("Smid", [128, 128], BF16, 4)
        hoo = R2("hoo", [128, 256], F32)
        hofw = R2("hofw", [128, 256], F32)
        gsl = R2("gsl", [128, 256], F32)
        hosq = R2("hosq", [128, 256], F32)
        hssq = R2("hssq", [128, 2], F32)
        hogo = R2("hogo", [128, 256], BF16)
        PS = []
        for dd_ in range(2):
            bA = P("bA%d" % dd_, [128, 512], F32)
            bB = P("bB%d" % dd_, [128, 512], F32)
            bC = P("bC%d" % dd_, [128, 512], F32)
            bD = P("bD%d" % dd_, [128, 512], F32)
            PS.append(dict(p_st=bA[:, 0:128], p_a=bA[:, 128:256], p_s=bA[:, 256:384], p_kt=bA[:, 384:512],
                           p_num=bB[:, 0:257], p_dc=bC[:, 0:257], p_x=bC[:, 257:391], p_b=bD[:, 0:256], p_o=bD[:, 256:512]))
        ktl32 = R2("ktl32", [128, 256], F32)
        identf = cn["ident"]
        d_hmb = nc.dram_tensor("d_hmb", [S, 256], F32)
        d_hob = nc.dram_tensor("d_hob", [S, 256], F32)
        for i in range(4):
            fw.op("pool", lambda e, i=i: e.memset(Qz[i][:], 0.0), [], [("Qz", i)])
        for d in range(2):
            fw.op("dve", lambda e, d=d: e.memset(Cst[d][:], 0.0), [], [("Cst", d)])
            for h in range(2):
                fw.op("dve", lambda e, d=d, h=h: e.memset(Sst[d][h][:], 0.0), [], [("Sst", d, h)])
        cnt = {"i": 0}
        for it_ in range(NT):
            for d in range(2):
                n = it_ if d == 0 else NT - 1 - it_
                mask_m = tri[d]
                hmask = cn["hmaskf"] if d == 0 else cn["hmaskb"]
                Mt = cn["Mf"] if d == 0 else cn["Mb"]
                Mx = cn["Mxf"] if d == 0 else cn["Mxb"]
                p_st = PS[d]["p_st"]; p_a = PS[d]["p_a"]; p_s = PS[d]["p_s"]; p_kt = PS[d]["p_kt"]
                p_num = PS[d]["p_num"]; p_dc = PS[d]["p_dc"]; p_x = PS[d]["p_x"]; p_b = PS[d]["p_b"]; p_o = PS[d]["p_o"]
                kd = lambda name, d=d: (name, d)
                r = d
                a_col = aa[:, d, n:n + 1]
                sc_col = sc[:, d, n:n + 1]
                th_col = thr[:, d, n:n + 1]
                fw.op("pe", lambda e, n=n: e.matmul(p_st, kT[:, n, :], qT[:, n, :], start=True, stop=True), [("kT", n), ("qT", n)], [("p_st", d)])
                fw.op("dve", lambda e, r=r, a_col=a_col, mask_m=mask_m: e.scalar_tensor_tensor(WT[r][:], p_st, a_col, mask_m[:], ALU.mult, ALU.mult), [("p_st", d), "aa", "c_trif", "c_trib"], [("WT", r)])
                fw.op("pool", lambda e, r=r, n=n, a_col=a_col: e.tensor_scalar(kp[r][:], ktm[:, n, :], a_col, None, ALU.mult), [("ktm", n), "aa"], [("kp", r)])
                fw.op("dve", lambda e, r=r, d=d, sc_col=sc_col: e.tensor_scalar(Csc32[r][:], Cst[d][:], sc_col, None, ALU.mult), [("Cst", d), "sc"], [("Csc32", r)])
                fw.op("act", lambda e, r=r: e.copy(Csc16[r][:], Csc32[r][:]), [("Csc32", r)], [("Csc16", r)])
                vk = [("vall", (n // 8) * 8), "vones"]
                fw.op("pe", lambda e, r=r, n=n: e.matmul(p_num, WT[r][:], vall[:, n, :], start=True, stop=False), [("WT", r)] + vk, [("p_num", d)])
                fw.op("pe", lambda e, r=r, n=n: e.matmul(p_num, qT[:, n, :], Csc16[r][:], start=False, stop=True), [("qT", n), ("Csc16", r)], [("p_num", d)])
                fw.op("pe", lambda e, r=r, n=n: e.matmul(p_dc, kp[r][:], vall[:, n, :], start=True, stop=True), [("kp", r)] + vk, [("p_dc", d)])
                fw.op("dve", lambda e, r=r, d=d: e.tensor_tensor(Cst[d][:], Csc32[r][:], p_dc, ALU.add), [("Csc32", r), ("p_dc", d)], [("Cst", d)])
                fw.op("act", lambda e, r=r: e.activation(den[r][:], p_num[:, 256:257], AF.Abs), [("p_num", d)], [("den", r)])
                fw.op("dve", lambda e, r=r, th_col=th_col: e.tensor_tensor(den[r][:], den[r][:], th_col, ALU.max), [("den", r), "thr"], [("den", r)])
                fw.op("dve", lambda e, r=r: e.reciprocal(den[r][:], den[r][:]), [("den", r)], [("den", r)])
                fw.op("act", lambda e, r=r: e.activation(hmo[r][:], p_num[:, 0:256], AF.Copy, scale=den[r][:]), [("p_num", d), ("den", r)], [("hmo", r)])
                fw.dma((d_hm if d == 0 else d_hmb)[n * 128:(n + 1) * 128, :], hmo[r][:], [("hmo", r)], [("d_hm", d, n)])
                fw.dma(fpre[r][:], d_tm[n * 128:(n + 1) * 128, 1024 + d * 256:1024 + (d + 1) * 256], [("d_tm", n)], [("fpre", r)])
                fw.dma(qin[r][:], d_fm[256:512, 2 + n * 128:2 + (n + 1) * 128].rearrange("(b p) t -> p b t", p=128), [("d_fm", n)], [("qin", r)])
                fw.dma(itl[r][:], d_tm[n * 128:(n + 1) * 128, 512:768], [("d_tm", n)], [("itl", r)], eng="pool")
                fw.op("act", lambda e, r=r: e.activation(ff[r][:], fpre[r][:], AF.Sigmoid), [("fpre", r)], [("ff", r)])
                fw.op("dve", lambda e, r=r, d=d: e.tensor_tensor(ff[r][:], ff[r][:], oml[:, d * 256:(d + 1) * 256], ALU.mult), [("ff", r), "oml"], [("ff", r)])
                fw.op("dve", lambda e, r=r, d=d: e.tensor_tensor(ff[r][:], ff[r][:], lb[:, d * 256:(d + 1) * 256], ALU.add), [("ff", r), "lb"], [("ff", r)])
                fw.op("act", lambda e, r=r: e.activation(logf[r][:], ff[r][:], AF.Ln), [("ff", r)], [("logf", r)])
                fw.op("pool", lambda e, r=r: e.tensor_scalar(kkk[r][:], ff[r][:], -1.0, 1.0, ALU.mult, ALU.add), [("ff", r)], [("kkk", r)])
                fw.op("pe", lambda e, r=r, Mt=Mt: e.matmul(p_b, Mt[:], logf[r][:], start=True, stop=True), [("logf", r), "c_Mf", "c_Mb"], [("p_b", d)])
                fw.op("act", lambda e, r=r: e.activation(eneg[r][:], p_b, AF.Exp, scale=-1.0), [("p_b", d)], [("eneg", r)])
                fw.op("dve", lambda e, r=r: e.tensor_tensor(ktl32[r][:], kkk[r][:], eneg[r][:], ALU.mult), [("kkk", r), ("eneg", r)], [("ktl32", r)])
                fw.op("pool", lambda e, r=r: e.tensor_copy(ktl[r][:], ktl32[r][:]), [("ktl32", r)], [("ktl", r)])
                for h in range(2):
                    q = cnt.setdefault("q", 0) % 4
                    cnt["q"] = cnt.get("q", 0) + 1
                    hs = slice(h * 128, (h + 1) * 128)
                    fw.op("pe", lambda e, r=r, hs=hs, Mx=Mx: e.matmul(p_x, logf[r][:, hs], Mx[:], start=True, stop=True), [("logf", r), "c_Mxf", "c_Mxb"], [("p_x", d)])
                    fw.op("act", lambda e, q=q: e.activation(Eq[q][:], p_x[:, 0:128], AF.Exp), [("p_x", d)], [("Eq", q)])
                    fw.op("act", lambda e, q=q: e.activation(ex[q][:], p_x[:, 128:134], AF.Exp), [("p_x", d)], [("ex", q)])
                    fw.op("dve", lambda e, q=q, r=r, h=h: e.tensor_tensor(Qz[q][:, 0, 0:64], qin[r][:, h, 0:64], Eq[q][:, 0:64], ALU.mult), [("qin", r), ("Eq", q)], [("Qz", q)])
                    fw.op("dve", lambda e, q=q, r=r, h=h: e.tensor_tensor(Qz[q][:, 1, 64:128], qin[r][:, h, 64:128], Eq[q][:, 64:128], ALU.mult), [("qin", r), ("Eq", q)], [("Qz", q)])
                    fw.op("pe", lambda e, r=r, hs=hs: e.transpose(p_kt, ktl32[r][:, hs], identf[:]), [("ktl32", r), "c_ident"], [("p_kt", d)])
                    fw.op("act", lambda e, q=q: e.copy(ktT[q][:], p_kt), [("p_kt", d)], [("ktT", q)])
                    fw.op("pe", lambda e, q=q: e.matmul(p_a[:, 0:64], ktT[q][:], Qz[q][:, 0, 0:64], start=True, stop=True), [("ktT", q), ("Qz", q)], [("p_a", d)])
                    fw.op("pe", lambda e, q=q: e.matmul(p_a[:, 64:128], ktT[q][:], Qz[q][:, 1, 64:128], start=True, stop=True), [("ktT", q), ("Qz", q)], [("p_a", d)])
                    fw.op("dve", lambda e, q=q, hmask=hmask: e.tensor_tensor(AT[q][:], p_a, hmask[:], ALU.mult), [("p_a", d), "c_hmaskf", "c_hmaskb"], [("AT", q)])
                    fw.op("pe", lambda e, q=q, r=r, hs=hs: e.matmul(p_o[:, hs], AT[q][:], itl[r][:, hs], start=True, stop=False), [("AT", q), ("itl", r)], [("p_o", d, h)])
                    corder = (0, 1) if d == 0 else (1, 0)
                    for ci_, c in enumerate(corder):
                        sm = cnt.setdefault("sm", 0) % 4
                        cnt["sm"] = cnt.get("sm", 0) + 1
                        fw.op("dve", lambda e, sm=sm, d=d, h=h, q=q, c=c: e.tensor_scalar(Smid[sm][:], Sst[d][h][:], ex[q][:, 3 * c:3 * c + 1], None, ALU.mult), [("Sst", d, h), ("ex", q)], [("Smid", sm)])
                        fw.op("pe", lambda e, sm=sm, q=q, c=c, hs=hs, ci_=ci_: e.matmul(p_o[:, hs], Qz[q][:, c, :], Smid[sm][:], start=False, stop=(ci_ == 1)), [("Qz", q), ("Smid", sm)], [("p_o", d, h)])
                        ps_ = slice(64 * c, 64 * c + 64)
                        fw.op("pe", lambda e, r=r, hs=hs, ps_=ps_: e.matmul(p_s, ktl[r][ps_, hs], itl[r][ps_, hs], start=True, stop=True), [("ktl", r), ("itl", r)], [("p_s", d)])
                        fw.op("dve", lambda e, d=d, h=h, q=q, c=c: e.tensor_scalar(Sst[d][h][:], Sst[d][h][:], ex[q][:, 3 * c + 1:3 * c + 2], None, ALU.mult), [("Sst", d, h), ("ex", q)], [("Sst", d, h)])
                        fw.op("dve", lambda e, d=d, h=h, q=q, c=c: e.scalar_tensor_tensor(Sst[d][h][:], p_s, ex[q][:, 3 * c + 2:3 * c + 3], Sst[d][h][:], ALU.mult, ALU.add), [("p_s", d), ("Sst", d, h), ("ex", q)], [("Sst", d, h)])
                fw.op("act", lambda e, r=r: e.copy(hoo[r][:], p_o), [("p_o", d, 0), ("p_o", d, 1)], [("hoo", r)])
                fw.dma((d_ho if d == 0 else d_hob)[n * 128:(n + 1) * 128, :], hoo[r][:], [("hoo", r)], [("d_ho", d, n)])

            if it_ % 16 == 15:
                fw.emit()
        hbw = R2("hbw", [128, 256], F32)
        hobw = R2("hobw", [128, 256], F32)
        for n in range(NT):
            r = n % 2
            rows = slice(n * 128, (n + 1) * 128)
            fw.dma(hfw[r][:], d_hm[rows, :], [("d_hm", 0, n)], [("hfw", r)])
            fw.dma(hbw[r][:], d_hmb[rows, :], [("d_hm", 1, n)], [("hbw", r)])
            fw.dma(osig[r][:], d_tm[rows, 256:512], [("d_tm", n)], [("osig", r)], eng="act")
            fw.dma(hofw[r][:], d_ho[rows, :], [("d_ho", 0, n)], [("hofw", r)])
            fw.dma(hobw[r][:], d_hob[rows, :], [("d_ho", 1, n)], [("hobw", r)])
            fw.dma(gsl[r][:], d_tm[rows, 768:1024], [("d_tm", n)], [("gsl", r)], eng="act")
            fw.op("dve", lambda e, r=r: e.tensor_tensor(hmo[r][:], hbw[r][:], hfw[r][:], ALU.add), [("hbw", r), ("hfw", r)], [("hmo", r)])
            fw.op("pool", lambda e, r=r: e.tensor_tensor(hsq[r][:], hmo[r][:], hmo[r][:], ALU.mult), [("hmo", r)], [("hsq", r)])
            fw.op("dve", lambda e, r=r: e.reduce_sum(ssq[r][:], hsq[r][:], AX.X), [("hsq", r)], [("ssq", r)])
            fw.op("dve", lambda e, r=r: e.tensor_scalar(ssq[r][:], ssq[r][:], 1.0 / 256, EPS, ALU.mult, ALU.add), [("ssq", r)], [("ssq", r)])
            fw.op("act", lambda e, r=r: e.activation(ssq[r][:], ssq[r][:], AF.Sqrt), [("ssq", r)], [("ssq", r)])
            fw.op("dve", lambda e, r=r: e.reciprocal(ssq[r][:], ssq[r][:]), [("ssq", r)], [("ssq", r)])
            fw.op("act", lambda e, r=r: e.activation(osig[r][:], osig[r][:], AF.Sigmoid), [("osig", r)], [("osig", r)])
            fw.op("dve", lambda e, r=r: e.scalar_tensor_tensor(hmo[r][:], hmo[r][:], ssq[r][:], gnm[:], ALU.mult, ALU.mult), [("hmo", r), ("ssq", r), "gnm"], [("hmo", r)])
            fw.op("dve", lambda e, r=r: e.tensor_tensor(hgo[r][:], hmo[r][:], osig[r][:], ALU.mult), [("hmo", r), ("osig", r)], [("hgo", r)])
            fw.dma(hg_out[rows, 0:256], hgo[r][:], [("hgo", r)], [("hg_out_m", n)])
            fw.op("dve", lambda e, r=r: e.tensor_tensor(hoo[r][:], hobw[r][:], hofw[r][:], ALU.add), [("hobw", r), ("hofw", r)], [("hoo", r)])
            fw.op("pool", lambda e, r=r: e.tensor_tensor(hosq[r][:], hoo[r][:], hoo[r][:], ALU.mult), [("hoo", r)], [("hosq", r)])
            fw.op("dve", lambda e, r=r: e.reduce_sum(hssq[r][:], hosq[r][:].rearrange("p (h v) -> p h v", h=2), AX.X), [("hosq", r)], [("hssq", r)])
            fw.op("dve", lambda e, r=r: e.tensor_scalar(hssq[r][:], hssq[r][:], 1.0 / 128, EPS, ALU.mult, ALU.add), [("hssq", r)], [("hssq", r)])
            fw.op("act", lambda e, r=r: e.activation(hssq[r][:], hssq[r][:], AF.Sqrt), [("hssq", r)], [("hssq", r)])
            fw.op("dve", lambda e, r=r: e.reciprocal(hssq[r][:], hssq[r][:]), [("hssq", r)], [("hssq", r)])
            fw.op("act", lambda e, r=r: e.activation(gsl[r][:], gsl[r][:], AF.Silu), [("gsl", r)], [("gsl", r)])
            for h in range(2):
                hs = slice(h * 128, (h + 1) * 128)
                fw.op("dve", lambda e, r=r, h=h, hs=hs: e.scalar_tensor_tensor(hoo[r][:, hs], hoo[r][:, hs], hssq[r][:, h:h + 1], gnh[:, hs], ALU.mult, ALU.mult), [("hoo", r), ("hssq", r), "gnh"], [("hoo", r)])
            fw.op("dve", lambda e, r=r: e.tensor_tensor(hogo[r][:], hoo[r][:], gsl[r][:], ALU.mult), [("hoo", r), ("gsl", r)], [("hogo", r)])
            fw.dma(hg_out[rows, 256:512], hogo[r][:], [("hogo", r)], [("hg_out_h", n)])
        fw.emit()


TQ = 2048
NTQ = TQ // 128


def transpose16(fw, src_bf, src_key, identb, pT, dst, dst_keys, n_chunks=16):
    for half in range(n_chunks // 8):
        pt = pT[half % 2]
        for j in range(8):
            k = half * 8 + j
            fw.op("pe", lambda e, pt=pt, j=j, k=k: e.transpose(pt[:, j, :], src_bf[:, k * 128:(k + 1) * 128], identb[:]), [src_key, "identb"], [("pT", half % 2)])
        if half % 2 == 0:
            fw.op("act", lambda e, pt=pt, half=half: e.copy(dst[:, half * 8:(half + 1) * 8, :], pt[:]), [("pT", half % 2)], [dst_keys[half]])
        else:
            fw.op("dve", lambda e, pt=pt, half=half: e.tensor_copy(dst[:, half * 8:(half + 1) * 8, :], pt[:]), [("pT", half % 2)], [dst_keys[half]])


def build_phase_b(nc, fw, io):
    C = io["consts"]
    ag1 = io["ag1"]
    d_h0 = nc.dram_tensor("d_h0", [TQ, D], F32)
    d_hT = nc.dram_tensor("d_hT", [NTQ, 128, 16 * 128], BF16)
    d_gm = nc.dram_tensor("d_gm", [TQ, D], BF16)
    d_gh = nc.dram_tensor("d_gh", [TQ, D], BF16)
    d_mT = nc.dram_tensor("d_mT", [NTQ, 128, 16 * 128], BF16)
    d_h1 = io["d_h1"]
    h1b_out = io["h1b_out"]
    aff_out = io["aff_out"]

    for sub in range(2):
        with ExitStack() as st:
            T = lambda n, sh, dt: st.enter_context(nc.sbuf_tensor("b%d_%s" % (sub, n), sh, dt))
            P = lambda n, sh, dt: st.enter_context(nc.psum_tensor("pb%d_%s" % (sub, n), sh, dt))
            wg = T("wg", [128, 16, D], BF16)
            bg = T("bg", [128, D], F32)
            for k in range(16):
                fw.dma(wg[:, k, :], io["w_g"][k * 128:(k + 1) * 128, sub * D:(sub + 1) * D], [], [("wg", k)], eng="pool")
            fw.dma(bg[:], io["b_g"][:, sub * D:(sub + 1) * D], [], ["bg"])
            hT = [T("hT%d" % i, [128, 16, 128], BF16) for i in range(2)]
            gt = [T("gt%d" % i, [128, D], BF16) for i in range(2)]
            gtmp = [T("gtmp%d" % i, [128, 512], F32) for i in range(2)]
            pg = [P("pg%d" % i, [128, 512], F32) for i in range(2)]
            if sub == 0:
                lng = T("lng", [128, D], F32)
                lnb = T("lnb", [128, D], F32)
                identb = T("identb", [128, 128], BF16)
                fw.dma(lng[:], io["ln_g"], [], ["lng"])
                fw.dma(lnb[:], io["ln_b"], [], ["lnb"])
                fw.dma(identb[:], C["ident"], [], ["identb"], eng="pool")
                xt = [T("xt%d" % i, [128, D], F32) for i in range(2)]
                sq = T("sq", [128, D], F32)
                h0 = [T("h0%d" % i, [128, D], F32) for i in range(2)]
                hb = T("hb", [128, D], BF16)
                small = [T("sm%d" % i, [128, 1], F32) for i in range(6)]
                pT = [P("pT%d" % i, [128, 8, 128], BF16) for i in range(2)]
            for t in range(NTQ):
                r = t % 2
                rows = slice(t * 128, (t + 1) * 128)
                if sub == 0:
                    fw.dma(xt[r][:], io["xq"][rows, :], [], [("xt", r)])
                    ln_rows(fw, T, xt[r][:], ("xt", r), lng[:], lnb[:], h0[r][:], ("h0", r), (sq,) + tuple(small))
                    fw.dma(d_h0[rows, :], h0[r][:], [("h0", r)], [("d_h0", t)])
                    fw.op("pool", lambda e, r=r: e.tensor_copy(hb[:], h0[r][:]), [("h0", r)], ["hb"])
                    transpose16(fw, hb, "hb", identb, pT, hT[r], [("hT", r, 0), ("hT", r, 1)])
                    fw.dma(d_hT[t], hT[r][:].rearrange("p a b -> p (a b)"), [("hT", r, 0), ("hT", r, 1)], [("d_hT", t)])
                else:
                    fw.dma(hT[r][:].rearrange("p a b -> p (a b)"), d_hT[t], [("d_hT", t)], [("hT", r, 0), ("hT", r, 1)])
                for cb in range(4):
                    pr = cb % 2
                    cs = slice(cb * 512, (cb + 1) * 512)
                    for k in range(16):
                        fw.op("pe", lambda e, pr=pr, k=k, r=r, cs=cs: e.matmul(pg[pr][:], hT[r][:, k, :], wg[:, k, cs], start=(k == 0), stop=(k == 15)),
                              [("hT", r, 0), ("hT", r, 1), ("wg", k)], [("pg", pr)])
                    fw.op("dve", lambda e, pr=pr, cs=cs: e.tensor_tensor(gtmp[pr][:], pg[pr][:], bg[:, cs], ALU.add), [("pg", pr), "bg"], [("gtmp", pr)])
                    fw.op("act", lambda e, pr=pr, r=r, cs=cs: e.activation(gt[r][:, cs], gtmp[pr][:], AF.Sigmoid), [("gtmp", pr)], [("gt", r, cb)])
                dst = d_gm if sub == 0 else d_gh
                fw.dma(dst[rows, :], gt[r][:], [("gt", r, cb) for cb in range(4)], [("d_g", sub, t)])
            fw.emit()

    with ExitStack() as st:
        T = lambda n, sh, dt: st.enter_context(nc.sbuf_tensor("b3_" + n, sh, dt))
        P = lambda n, sh, dt: st.enter_context(nc.psum_tensor("pb3_" + n, sh, dt))
        wbm = T("wbm", [128, 8, D], BF16)
        wbh = T("wbh", [128, 8, D], BF16)
        for k in range(8):
            fw.dma(wbm[:, k, :], io["w_bm"][k * 128:(k + 1) * 128, :], [], [("wbm", k)], eng="pool")
            fw.dma(wbh[:, k, :], io["w_bh"][k * 128:(k + 1) * 128, :], [], [("wbh", k)], eng="pool")
        identb = T("identb", [128, 128], BF16)
        fw.dma(identb[:], C["ident"], [], ["identb"], eng="pool")
        gidx = T("gidx", [128, NTQ, 4], I32)
        fw.dma(gidx[:], io["gidx"], [], ["gidx"])
        hgt = [T("hgt%d" % i, [128, 4, 512], BF16) for i in range(2)]
        hgT = [T("hgT%d" % i, [128, 16, 128], BF16) for i in range(2)]
        gm = [T("gm%d" % i, [128, D], BF16) for i in range(2)]
        gh = [T("gh%d" % i, [128, D], BF16) for i in range(2)]
        t1 = [T("t1%d" % i, [128, 512], F32) for i in range(2)]
        t2 = [T("t2%d" % i, [128, 512], F32) for i in range(2)]
        mg = [T("mg%d" % i, [128, D], BF16) for i in range(2)]
        mT = [T("mT%d" % i, [128, 16, 128], BF16) for i in range(2)]
        pT = [P("pT%d" % i, [128, 8, 128], BF16) for i in range(2)]
        pym = [P("pym%d" % i, [128, 512], F32) for i in range(2)]
        pyh = [P("pyh%d" % i, [128, 512], F32) for i in range(2)]
        for t in range(NTQ):
            r = t % 2
            rows = slice(t * 128, (t + 1) * 128)
            for hg in range(4):
                fw.op("pool", lambda e, r=r, hg=hg, t=t: e.indirect_dma_start(out=hgt[r][:, hg, :], out_offset=None, in_=ag1,
                                                                               in_offset=bass.IndirectOffsetOnAxis(ap=gidx[:, t, hg:hg + 1], axis=0)),
                      ["gidx", "ag1"], [("hgt", r, hg)], kind="d")
            fw.dma(gm[r][:], d_gm[rows, :], [("d_g", 0, t)], [("gm", r)])
            fw.dma(gh[r][:], d_gh[rows, :], [("d_g", 1, t)], [("gh", r)])
            for half in range(2):
                pt = pT[half]
                for j in range(8):
                    kk = half * 8 + j
                    hg = (kk % 8) // 2
                    off = (0 if kk < 8 else 256) + (kk % 2) * 128
                    fw.op("pe", lambda e, pt=pt, j=j, hg=hg, off=off, r=r: e.transpose(pt[:, j, :], hgt[r][:, hg, off:off + 128], identb[:]),
                          [("hgt", r, hg), "identb"], [("pT", half)])
                if half == 0:
                    fw.op("act", lambda e, pt=pt, r=r: e.copy(hgT[r][:, 0:8, :], pt[:]), [("pT", 0)], [("hgT", r, 0)])
                else:
                    fw.op("dve", lambda e, pt=pt, r=r: e.tensor_copy(hgT[r][:, 8:16, :], pt[:]), [("pT", 1)], [("hgT", r, 1)])
            for cb in range(4):
                pr = cb % 2
                cs = slice(cb * 512, (cb + 1) * 512)
                for k in range(8):
                    fw.op("pe", lambda e, pr=pr, k=k, r=r, cs=cs: e.matmul(pym[pr][:], hgT[r][:, k, :], wbm[:, k, cs], start=(k == 0), stop=(k == 7)),
                          [("hgT", r, 0), ("wbm", k)], [("pym", pr)])
                for k in range(8):
                    fw.op("pe", lambda e, pr=pr, k=k, r=r, cs=cs: e.matmul(pyh[pr][:], hgT[r][:, 8 + k, :], wbh[:, k, cs], start=(k == 0), stop=(k == 7)),
                          [("hgT", r, 1), ("wbh", k)], [("pyh", pr)])
                fw.op("dve", lambda e, pr=pr, r=r, cs=cs: e.tensor_tensor(t1[pr][:], pym[pr][:], gm[r][:, cs], ALU.mult), [("pym", pr), ("gm", r)], [("t1", pr)])
                fw.op("dve", lambda e, pr=pr, r=r, cs=cs: e.tensor_tensor(t2[pr][:], pyh[pr][:], gh[r][:, cs], ALU.mult), [("pyh", pr), ("gh", r)], [("t2", pr)])
                fw.op("pool", lambda e, pr=pr, r=r, cs=cs: e.tensor_tensor(mg[r][:, cs], t1[pr][:], t2[pr][:], ALU.add), [("t1", pr), ("t2", pr)], [("mg", r)])
            transpose16(fw, mg[r], ("mg", r), identb, pT, mT[r], [("mT", r, 0), ("mT", r, 1)])
            fw.dma(d_mT[t], mT[r][:].rearrange("p a b -> p (a b)"), [("mT", r, 0), ("mT", r, 1)], [("d_mT", t)])
        fw.emit()

    with ExitStack() as st:
        T = lambda n, sh, dt: st.enter_context(nc.sbuf_tensor("b4_" + n, sh, dt))
        P = lambda n, sh, dt: st.enter_context(nc.psum_tensor("pb4_" + n, sh, dt))
        wo = T("wo", [128, 16, D], BF16)
        for k in range(16):
            fw.dma(wo[:, k, :], io["w_out"][k * 128:(k + 1) * 128, :], [], [("wo", k)], eng="pool")
        wr = T("wr", [128, 16, 16], F32)
        fw.dma(wr[:], io["w_router"].rearrange("(k p) e -> p k e", p=128), [], ["wr"])
        identf = T("identf", [128, 128], F32)
        fw.dma(identf[:], C["ident"], [], ["identf"])
        lng = T("lng", [128, D], F32)
        lnb = T("lnb", [128, D], F32)
        fw.dma(lng[:], io["ln1_g"], [], ["lng"])
        fw.dma(lnb[:], io["ln1_b"], [], ["lnb"])
        mT = [T("mT%d" % i, [128, 16, 128], BF16) for i in range(2)]
        h0 = [T("h0%d" % i, [128, D], F32) for i in range(2)]
        pre = [T("pre%d" % i, [128, D], F32) for i in range(2)]
        sq = T("sq", [128, D], F32)
        small = [T("sm%d" % i, [128, 1], F32) for i in range(6)]
        h1 = [T("h1%d" % i, [128, D], F32) for i in range(2)]
        h1b = [T("h1b%d" % i, [128, D], BF16) for i in range(2)]
        h1T = T("h1T", [128, 16, 128], F32)
        lg = [T("lg%d" % i, [128, 16], F32) for i in range(2)]
        mx = [T("mx%d" % i, [128, 1], F32) for i in range(2)]
        sm_ = [T("sms%d" % i, [128, 1], F32) for i in range(2)]
        pm = [P("pm%d" % i, [128, 512], F32) for i in range(2)]
        pTf = [P("pTf%d" % i, [128, 4, 128], F32) for i in range(2)]
        pl = P("pl", [128, 16], F32)
        for t in range(NTQ):
            r = t % 2
            rows = slice(t * 128, (t + 1) * 128)
            fw.dma(mT[r][:].rearrange("p a b -> p (a b)"), d_mT[t], [("d_mT", t)], [("mT", r)])
            fw.dma(h0[r][:], d_h0[rows, :], [("d_h0", t)], [("h0", r)])
            for cb in range(4):
                pr = cb % 2
                cs = slice(cb * 512, (cb + 1) * 512)
                for k in range(16):
                    fw.op("pe", lambda e, pr=pr, k=k, r=r, cs=cs: e.matmul(pm[pr][:], mT[r][:, k, :], wo[:, k, cs], start=(k == 0), stop=(k == 15)),
                          [("mT", r), ("wo", k)], [("pm", pr)])
                fw.op("dve", lambda e, pr=pr, r=r, cs=cs: e.scalar_tensor_tensor(pre[r][:, cs], h0[r][:, cs], ALPHA, pm[pr][:], ALU.mult, ALU.add), [("h0", r), ("pm", pr)], [("pre", r)])
            ln_rows(fw, T, pre[r][:], ("pre", r), lng[:], lnb[:], h1[r][:], ("h1", r), (sq,) + tuple(small))
            fw.dma(d_h1[rows, :], h1[r][:], [("h1", r)], [("d_h1", t)])
            fw.op("pool", lambda e, r=r: e.tensor_copy(h1b[r][:], h1[r][:]), [("h1", r)], [("h1b", r)])
            fw.dma(h1b_out[rows, :], h1b[r][:], [("h1b", r)], [("h1b_out", t)])
            for g4 in range(4):
                pt = pTf[g4 % 2]
                for j in range(4):
                    k = g4 * 4 + j
                    fw.op("pe", lambda e, pt=pt, j=j, k=k, r=r: e.transpose(pt[:, j, :], h1[r][:, k * 128:(k + 1) * 128], identf[:]), [("h1", r), "identf"], [("pTf", g4 % 2)])
                fw.op("act" if g4 % 2 == 0 else "dve", (lambda e, pt=pt, g4=g4: e.copy(h1T[:, g4 * 4:(g4 + 1) * 4, :], pt[:])) if g4 % 2 == 0 else
                      (lambda e, pt=pt, g4=g4: e.tensor_copy(h1T[:, g4 * 4:(g4 + 1) * 4, :], pt[:])), [("pTf", g4 % 2)], [("h1T", g4)])
            for k in range(16):
                fw.op("pe", lambda e, k=k: e.matmul(pl[:], h1T[:, k, :], wr[:, k, :], start=(k == 0), stop=(k == 15)), [("h1T", k // 4), "wr"], ["pl"])
            fw.op("dve", lambda e, r=r: e.reduce_max(mx[r][:], pl[:], AX.X), ["pl"], [("mx", r)])
            fw.op("dve", lambda e, r=r: e.tensor_scalar(mx[r][:], mx[r][:], -1.0, None, ALU.mult), [("mx", r)], [("mx", r)])
            fw.op("act", lambda e, r=r: e.activation(lg[r][:], pl[:], AF.Exp, bias=mx[r][:]), ["pl", ("mx", r)], [("lg", r)])
            fw.op("dve", lambda e, r=r: e.reduce_sum(sm_[r][:], lg[r][:], AX.X), [("lg", r)], [("sms", r)])
            fw.op("dve", lambda e, r=r: e.reciprocal(sm_[r][:], sm_[r][:]), [("sms", r)], [("sms", r)])
            fw.op("dve", lambda e, r=r: e.tensor_scalar(lg[r][:], lg[r][:], sm_[r][:], None, ALU.mult), [("lg", r), ("sms", r)], [("lg", r)])
            fw.dma(aff_out[rows, :], lg[r][:], [("lg", r)], [("aff_out", t)])
        fw.emit()


DFF = 5632
NFB = DFF // 128
CAP = 1024
TQ = 2048
NTQ = 16
NBIS = 34

C_CONST_SHAPES = {"sl": [128, 128], "iota": [128, 1024], "tokinfo": [128, 64, 2]}


def host_consts_c():
    s = np.arange(128)[:, None]
    t = np.arange(128)[None, :]
    c = {}
    c["sl"] = (s < t).astype(np.float32)
    c["iota"] = np.ascontiguousarray(np.broadcast_to(np.arange(1024, dtype=np.float32)[None, :], (128, 1024)))
    ti = np.zeros((128, 64, 2), np.float32)
    ti[:, :, 0] = np.arange(128)[:, None]
    ti[:, :, 1] = np.arange(64)[None, :]
    c["tokinfo"] = ti
    return c


def build_phase_c(nc, fw, io):
    C = io["consts"]
    ag2 = io["ag2"]
    ag_aff = io["ag_aff"]
    ye_out = io["ye_out"]
    idx_out = io["idx_out"]
    d_idx = nc.dram_tensor("d_idx", [128, 4, 8], I32)
    d_gsel = nc.dram_tensor("d_gsel", [128, 4, 8], F32)

    with ExitStack() as st:
        T = lambda n, sh, dt: st.enter_context(nc.sbuf_tensor("c0_" + n, sh, dt))
        P = lambda n, sh, dt: st.enter_context(nc.psum_tensor("pc0_" + n, sh, dt))
        ones = T("ones", [128, 128], F32)
        sl = T("sl", [128, 128], F32)
        iota = T("iota", [128, 1024], F32)
        tokinfo = T("tokinfo", [128, 64, 2], BF16)
        selm = T("selm", [128, 2, 1, 16], F32)
        fw.dma(ones[:], C["ones"], [], ["ones"])
        fw.dma(sl[:], C["sl"], [], ["sl"])
        fw.dma(iota[:], C["iota"], [], ["iota"])
        fw.dma(tokinfo[:], C["tokinfo"], [], ["tokinfo"], eng="pool")
        fw.dma(selm[:], io["selm"], [], ["selm"])
        affb = [T("affb%d" % i, [128, 64, 16], F32) for i in range(2)]
        tmp = T("tmp", [128, 64, 16], F32)
        vals = T("vals", [128, 4, 64], F32)
        lo = T("lo", [128, 4], F32)
        hi = T("hi", [128, 4], F32)
        mid = T("mid", [128, 4], F32)
        cmp = [T("cmp%d" % i, [128, 64], F32) for i in range(2)]
        cntp = T("cntp", [128, 4], F32)
        ge = T("ge", [128, 4], F32)
        dd = T("dd", [128, 4], F32)
        d2 = T("d2", [128, 4], F32)
        m = T("m", [128, 4, 64], F32)
        onesrow = T("onesrow", [128, 64], F32)
        incl = T("incl", [128, 4, 64], F32)
        pos = T("pos", [128, 4, 64], F32)
        rowtot = T("rowtot", [128, 4], F32)
        offs = T("offs", [128, 4], F32)
        oneh = [T("oneh%d" % i, [128, 1024], BF16) for i in range(2)]
        idxf = T("idxf", [128, 4, 8], F32)
        pidxs = T("pidxs", [128, 8, 2], F32)
        idxi = T("idxi", [128, 4, 8], I32)
        ptot = P("ptot", [128, 4], F32)
        poffs = P("poffs", [128, 4], F32)
        pidx = P("pidx", [128, 8, 2], F32)
        for b in range(2):
            fw.dma(affb[b][:], ag_aff[b * S:(b + 1) * S, :].rearrange("(p f) e -> p f e", p=128), ["ag_aff"], [("affb", b)])
            for el in range(2):
                L = b * 2 + el
                fw.op("dve", lambda e, b=b, el=el: e.tensor_tensor(tmp[:], affb[b][:], selm[:, el, :, :].to_broadcast([128, 64, 16]), ALU.mult), [("affb", b), "selm"], ["tmp"])
                fw.op("dve", lambda e, L=L: e.reduce_sum(vals[:, L, :], tmp[:], AX.X), ["tmp"], ["vals"])
        fw.op("dve", lambda e: e.memset(lo[:], 0.0), [], ["lo"])
        fw.op("dve", lambda e: e.memset(hi[:], 1.0), [], ["hi"])
        fw.op("dve", lambda e: e.memset(onesrow[:], 1.0), [], ["onesrow"])
        for it in range(NBIS):
            fw.op("dve", lambda e: e.tensor_tensor(mid[:], lo[:], hi[:], ALU.add), ["lo", "hi"], ["mid"])
            fw.op("dve", lambda e: e.tensor_scalar(mid[:], mid[:], 0.5, None, ALU.mult), ["mid"], ["mid"])
            for L in range(4):
                fw.op("dve", lambda e, L=L: e.tensor_scalar(cmp[L % 2][:], vals[:, L, :], mid[:, L:L + 1], None, ALU.is_gt), ["vals", "mid"], [("cmp", L % 2)])
                fw.op("dve", lambda e, L=L: e.reduce_sum(cntp[:, L:L + 1], cmp[L % 2][:], AX.X), [("cmp", L % 2)], ["cntp"])
            fw.op("pe", lambda e: e.matmul(ptot[:], ones[:], cntp[:], start=True, stop=True), ["ones", "cntp"], ["ptot"])
            fw.op("dve", lambda e: e.tensor_scalar(ge[:], ptot[:], CAP - 0.5, None, ALU.is_ge), ["ptot"], ["ge"])
            fw.op("dve", lambda e: e.tensor_tensor(dd[:], mid[:], lo[:], ALU.subtract), ["mid", "lo"], ["dd"])
            fw.op("dve", lambda e: e.tensor_tensor(dd[:], dd[:], ge[:], ALU.mult), ["dd", "ge"], ["dd"])
            fw.op("dve", lambda e: e.tensor_tensor(d2[:], hi[:], mid[:], ALU.subtract), ["mid", "hi"], ["d2"])
            fw.op("dve", lambda e: e.tensor_tensor(d2[:], d2[:], ge[:], ALU.mult), ["d2", "ge"], ["d2"])
            fw.op("dve", lambda e: e.tensor_tensor(lo[:], lo[:], dd[:], ALU.add), ["lo", "dd"], ["lo"])
            fw.op("dve", lambda e: e.tensor_tensor(hi[:], mid[:], d2[:], ALU.add), ["mid", "d2"], ["hi"])
        for L in range(4):
            fw.op("dve", lambda e, L=L: e.tensor_scalar(m[:, L, :], vals[:, L, :], lo[:, L:L + 1], None, ALU.is_gt), ["vals", "lo"], ["m"])
            fw.op("dve", lambda e, L=L: e.tensor_tensor_scan(incl[:, L, :], onesrow[:], m[:, L, :], 0.0, ALU.mult, ALU.add), ["m", "onesrow"], ["incl"])
            fw.op("dve", lambda e, L=L: e.tensor_copy(rowtot[:, L:L + 1], incl[:, L, 63:64]), ["incl"], ["rowtot"])
        fw.op("pe", lambda e: e.matmul(poffs[:], sl[:], rowtot[:], start=True, stop=True), ["sl", "rowtot"], ["poffs"])
        fw.op("dve", lambda e: e.tensor_copy(offs[:], poffs[:]), ["poffs"], ["offs"])
        fw.op("dve", lambda e: e.tensor_tensor(pos[:], incl[:], m[:], ALU.subtract), ["incl", "m"], ["pos"])
        for L in range(4):
            fw.op("dve", lambda e, L=L: e.tensor_scalar(pos[:, L, :], pos[:, L, :], offs[:, L:L + 1], None, ALU.add), ["pos", "offs"], ["pos"])
        zb = T("zb", [128, 128], BF16)
        fw.op("pool", lambda e: e.memset(zb[:], 0.0), [], ["zb"])
        for L in range(4):
            b = L // 2
            fw.op("pe", lambda e: e.matmul(pidx[:].rearrange("p a b -> p (a b)"), zb[:], tokinfo[:, 0:8, :].rearrange("p a b -> p (a b)"), start=True, stop=False), ["zb", "tokinfo", "pidxs"], ["pidx"])
            for f in range(64):
                r = f % 2
                fw.op("dve", lambda e, L=L, f=f, r=r: e.tensor_scalar(oneh[r][:], iota[:], pos[:, L, f:f + 1], m[:, L, f:f + 1], ALU.is_equal, ALU.mult), ["iota", "pos", "m"], [("oneh", r)])
                for jb in range(8):
                    fw.op("pe", lambda e, jb=jb, f=f, r=r: e.matmul(pidx[:, jb, :], oneh[r][:, jb * 128:(jb + 1) * 128], tokinfo[:, f, :], start=False, stop=(f == 63)),
                          [("oneh", r), "tokinfo"], ["pidx"])
            fw.op("dve", lambda e: e.tensor_copy(pidxs[:], pidx[:]), ["pidx"], ["pidxs"])
            fw.op("dve", lambda e, L=L: e.scalar_tensor_tensor(idxf[:, L, :], pidxs[:, :, 0], 64.0, pidxs[:, :, 1], ALU.mult, ALU.add), ["pidxs"], [("idxf", L)])
            fw.op("dve", lambda e, L=L, b=b: e.tensor_scalar(idxf[:, L, :], idxf[:, L, :], float(b * S), None, ALU.add), [("idxf", L)], [("idxf", L)])
        fw.op("dve", lambda e: e.tensor_copy(idxi[:], idxf[:]), [("idxf", L) for L in range(4)], ["idxi"])
        fw.dma(d_idx[:, :, :], idxi[:], ["idxi"], ["d_idx"])
        for L in range(4):
            fw.dma(idx_out[L * CAP:(L + 1) * CAP, :].rearrange("(j p) o -> p (j o)", p=128), idxi[:, L, :], ["idxi"], [("idx_out", L)], allow_slow_non_contiguous=True)
        fw.emit()

    with ExitStack() as st0:
        T0 = lambda n, sh, dt: st0.enter_context(nc.sbuf_tensor("c1_" + n, sh, dt))
        hidT = T0("hidT", [128, NFB, CAP], BF16)
        idxi = T0("idxi", [128, 4, 8], I32)
        selm = T0("selm", [128, 2, 1, 16], F32)
        identb = T0("identb", [128, 128], BF16)
        gsel = T0("gsel", [128, 4, 8], F32)
        fw.dma(idxi[:], d_idx[:, :, :], ["d_idx"], ["idxi1"])
        fw.dma(selm[:], io["selm"], [], ["selm1"])
        fw.dma(identb[:], C["ident"], [], ["identb"], eng="pool")
        for L in range(4):
            b, el = L // 2, L % 2
            with ExitStack() as st:
                T = lambda n, sh, dt: st.enter_context(nc.sbuf_tensor("c1a%d_%s" % (L, n), sh, dt))
                P = lambda n, sh, dt: st.enter_context(nc.psum_tensor("pc1a%d_%s" % (L, n), sh, dt))
                xe = [T("xe%d" % i, [128, D], BF16) for i in range(2)]
                xeT = T("xeT", [128, 16, CAP], BF16)
                gat = T("gat", [128, 8, 16], F32)
                g16 = T("g16", [128, 16], F32)
                wgt = [T("wgt%d" % i, [128, 16, 128], BF16) for i in range(2)]
                wut = [T("wut%d" % i, [128, 16, 128], BF16) for i in range(2)]
                sg = [T("sg%d" % i, [128, 512], F32) for i in range(2)]
                pT = [P("pT%d" % i, [128, 8, 128], BF16) for i in range(2)]
                pgt = [P("pgt%d" % i, [128, 512], F32) for i in range(2)]
                put = [P("put%d" % i, [128, 512], F32) for i in range(2)]
                for jb in range(8):
                    r = jb % 2
                    fw.op("pool", lambda e, r=r, jb=jb, L=L: e.indirect_dma_start(out=xe[r][:], out_offset=None, in_=ag2,
                                                                                   in_offset=bass.IndirectOffsetOnAxis(ap=idxi[:, L, jb:jb + 1], axis=0)),
                          ["idxi1", "ag2"], [("xe", r)], kind="d")
                    fw.op("pool", lambda e, jb=jb, L=L: e.indirect_dma_start(out=gat[:, jb, :], out_offset=None, in_=ag_aff,
                                                                              in_offset=bass.IndirectOffsetOnAxis(ap=idxi[:, L, jb:jb + 1], axis=0)),
                          ["idxi1", "ag_aff"], [("gat", jb)], kind="d")
                    for half in range(2):
                        pt = pT[half]
                        for j in range(8):
                            k = half * 8 + j
                            fw.op("pe", lambda e, pt=pt, j=j, k=k, r=r: e.transpose(pt[:, j, :], xe[r][:, k * 128:(k + 1) * 128], identb[:]), [("xe", r), "identb"], [("pT", half)])
                        if half == 0:
                            fw.op("act", lambda e, pt=pt, jb=jb: e.copy(xeT[:, 0:8, jb * 128:(jb + 1) * 128], pt[:]), [("pT", 0)], [("xeT", jb, 0)])
                        else:
                            fw.op("dve", lambda e, pt=pt, jb=jb: e.tensor_copy(xeT[:, 8:16, jb * 128:(jb + 1) * 128], pt[:]), [("pT", 1)], [("xeT", jb, 1)])
                    fw.op("dve", lambda e, jb=jb, el=el: e.tensor_tensor(g16[:], gat[:, jb, :], selm[:, el, 0, :], ALU.mult), [("gat", jb), "selm1"], ["g16"])
                    fw.op("dve", lambda e, jb=jb, L=L: e.reduce_sum(gsel[:, L, jb:jb + 1], g16[:], AX.X), ["g16"], [("gsel", L, jb)])
                xk = [("xeT", jb, h) for jb in range(8) for h in range(2)]
                for fb in range(NFB):
                    r = fb % 2
                    fs = slice(fb * 128, (fb + 1) * 128)
                    fw.dma(wgt[r][:], io["w_gate"][el, :, fs].rearrange("(k p) f -> p k f", p=128), [], [("wgt", r)], eng="pool")
                    fw.dma(wut[r][:], io["w_up"][el, :, fs].rearrange("(k p) f -> p k f", p=128), [], [("wut", r)], eng="pool")
                    for tb in range(2):
                        ts_ = slice(tb * 512, (tb + 1) * 512)
                        for k in range(16):
                            fw.op("pe", lambda e, tb=tb, k=k, r=r, ts_=ts_: e.matmul(pgt[tb][:], wgt[r][:, k, :], xeT[:, k, ts_], start=(k == 0), stop=(k == 15)),
                                  [("wgt", r)] + (xk if fb == 0 else []), [("pgt", tb)])
                        for k in range(16):
                            fw.op("pe", lambda e, tb=tb, k=k, r=r, ts_=ts_: e.matmul(put[tb][:], wut[r][:, k, :], xeT[:, k, ts_], start=(k == 0), stop=(k == 15)),
                                  [("wut", r)], [("put", tb)])
                        fw.op("act", lambda e, tb=tb: e.activation(sg[tb][:], pgt[tb][:], AF.Silu), [("pgt", tb)], [("sg", tb)])
                        fw.op("dve", lambda e, tb=tb, fb=fb, ts_=ts_: e.tensor_tensor(hidT[:, fb, ts_], sg[tb][:], put[tb][:], ALU.mult), [("sg", tb), ("put", tb)], [("hidT", fb)])
                fw.emit()
            with ExitStack() as st:
                T = lambda n, sh, dt: st.enter_context(nc.sbuf_tensor("c1b%d_%s" % (L, n), sh, dt))
                P = lambda n, sh, dt: st.enter_context(nc.psum_tensor("pc1b%d_%s" % (L, n), sh, dt))
                wd = [T("wd%d" % i, [128, NFB, 256], BF16) for i in range(2)]
                yo = [T("yo%d" % i, [128, 256], F32) for i in range(2)]
                py = [P("py%d" % i, [128, 256], F32) for i in range(2)]
                hk = [("hidT", fb) for fb in range(NFB)]
                cnt = 0
                for dmb in range(8):
                    r = dmb % 2
                    cs = slice(dmb * 256, (dmb + 1) * 256)
                    for hf in range(2):
                        fw.dma(wd[r][:, hf * 22:(hf + 1) * 22, :], io["w_down"][el, hf * 22 * 128:(hf + 1) * 22 * 128, cs].rearrange("(k p) c -> p k c", p=128), [], [("wd", r, hf)], eng="pool")
                    for jb in range(8):
                        q = cnt % 2
                        cnt += 1
                        for fc in range(NFB):
                            fw.op("pe", lambda e, q=q, fc=fc, jb=jb, r=r: e.matmul(py[q][:], hidT[:, fc, jb * 128:(jb + 1) * 128], wd[r][:, fc, :], start=(fc == 0), stop=(fc == NFB - 1)),
                                  [("wd", r, fc // 22)] + (hk if ((dmb == 0 and jb == 0 and fc == 0) or (dmb == 7 and jb == 7 and fc == NFB - 1)) else []), [("py", q)])
                        fw.op("act", lambda e, q=q, jb=jb, L=L: e.activation(yo[q][:], py[q][:], AF.Copy, scale=gsel[:, L, jb:jb + 1]), [("py", q), ("gsel", L, jb)], [("yo", q)])
                        fw.dma(ye_out[L * CAP + jb * 128:L * CAP + (jb + 1) * 128, cs], yo[q][:], [("yo", q)], [("ye_out", L, jb, dmb)])
                fw.emit()


def build_phase_d(nc, fw, io):
    C = io["consts"]
    ag3 = io["ag3"]
    ag_idx = io["ag_idx"]
    d_h1 = io["d_h1"]
    out = io["out"]
    ffn = nc.dram_tensor("ffn_acc", [TQ + 128, D], F32)
    NG = 128
    with ExitStack() as st:
        T = lambda n, sh, dt: st.enter_context(nc.sbuf_tensor("d_" + n, sh, dt))
        zero = T("zero", [128, D], F32)
        fw.op("pool", lambda e: e.memset(zero[:], 0.0), [], ["zero"])
        for t in range(NTQ):
            fw.dma(ffn[t * 128:(t + 1) * 128, :], zero[:], ["zero"], ["ffn"])
        drow = T("drow", [128, NG], I32)
        basef = T("basef", [128, 1], F32)
        fw.dma(drow[:], io["drow"], [], ["drow"])
        fw.dma(basef[:], io["basef"], [], ["basef"])
        trash = T("trash", [128, 1], F32)
        fw.dma(trash[:], io["trash"], [], ["trash"])
        toki = T("toki", [128, NG], I32)
        tokf = T("tokf", [128, NG], F32)
        drowf = T("drowf", [128, NG], F32)
        v1 = T("v1", [128, NG], F32)
        v2 = T("v2", [128, NG], F32)
        idl = T("idl", [128, NG], F32)
        idli = T("idli", [128, NG], I32)
        grf = T("grf", [128, NG], F32)
        gri = T("gri", [128, NG], I32)
        yg = [T("yg%d" % i, [128, D], F32) for i in range(3)]
        idc = [T("idc%d" % i, [128, 1], I32) for i in range(3)]
        for g in range(NG):
            fw.op("pool", lambda e, g=g: e.indirect_dma_start(out=toki[:, g:g + 1], out_offset=None, in_=ag_idx,
                                                               in_offset=bass.IndirectOffsetOnAxis(ap=drow[:, g:g + 1], axis=0)),
                  ["drow", "ag_idx"], [("toki", g)], kind="d")
        tk = [("toki", g) for g in range(NG)]
        BIG = 100000.0
        BIGR = 1000000.0
        fw.op("dve", lambda e: e.tensor_copy(tokf[:], toki[:]), tk, ["tokf"])
        fw.op("dve", lambda e: e.tensor_copy(drowf[:], drow[:]), ["drow"], ["drowf"])
        fw.op("dve", lambda e: e.tensor_scalar(tokf[:], tokf[:], basef[:, 0:1], None, ALU.subtract), ["tokf", "basef"], ["tokf"])
        fw.op("dve", lambda e: e.tensor_scalar(v1[:], tokf[:], -0.5, None, ALU.is_ge), ["tokf"], ["v1"])
        fw.op("dve", lambda e: e.tensor_scalar(v2[:], tokf[:], TQ - 0.5, None, ALU.is_lt), ["tokf"], ["v2"])
        fw.op("dve", lambda e: e.tensor_tensor(v1[:], v1[:], v2[:], ALU.mult), ["v1", "v2"], ["v1"])
        fw.op("dve", lambda e: e.tensor_scalar(idl[:], tokf[:], trash[:, 0:1], None, ALU.subtract), ["tokf", "trash"], ["idl"])
        fw.op("dve", lambda e: e.tensor_tensor(idl[:], idl[:], v1[:], ALU.mult), ["idl", "v1"], ["idl"])
        fw.op("dve", lambda e: e.tensor_scalar(idl[:], idl[:], trash[:, 0:1], None, ALU.add), ["idl", "trash"], ["idl"])
        fw.op("dve", lambda e: e.tensor_copy(idli[:], idl[:]), ["idl"], ["idli"])
        for g in range(NG):
            r = g % 3
            fw.op("pool", lambda e, g=g, r=r: e.indirect_dma_start(out=yg[r][:], out_offset=None, in_=ag3,
                                                                    in_offset=bass.IndirectOffsetOnAxis(ap=drow[:, g:g + 1], axis=0)),
                  ["drow", "ag3"], [("yg", r)], kind="d")
            fw.op("dve", lambda e, g=g, r=r: e.tensor_copy(idc[r][:], idli[:, g:g + 1]), ["idli"], [("idc", r)])
            fw.op("pool", lambda e, g=g, r=r: e.indirect_dma_start(out=ffn[:, :], out_offset=bass.IndirectOffsetOnAxis(ap=idc[r][:, :], axis=0),
                                                                    in_=yg[r][:], in_offset=None, compute_op=ALU.add),
                  [("idc", r), ("yg", r), "ffn"], ["ffn"], kind="d")
            if g % 16 == 15:
                fw.emit()
        fw.emit()
    with ExitStack() as st:
        T = lambda n, sh, dt: st.enter_context(nc.sbuf_tensor("d2_" + n, sh, dt))
        lng = T("lng", [128, D], F32)
        lnb = T("lnb", [128, D], F32)
        fw.dma(lng[:], io["ln2_g"], [], ["lng"])
        fw.dma(lnb[:], io["ln2_b"], [], ["lnb"])
        h1 = [T("h1%d" % i, [128, D], F32) for i in range(2)]
        ff = [T("ff%d" % i, [128, D], F32) for i in range(2)]
        pre = [T("pre%d" % i, [128, D], F32) for i in range(2)]
        oo = [T("oo%d" % i, [128, D], F32) for i in range(2)]
        sq = T("sq", [128, D], F32)
        small = [T("sm%d" % i, [128, 1], F32) for i in range(6)]
        for t in range(NTQ):
            r = t % 2
            rows = slice(t * 128, (t + 1) * 128)
            fw.dma(h1[r][:], d_h1[rows, :], [("d_h1", t)], [("h1", r)])
            fw.dma(ff[r][:], ffn[rows, :], ["ffn"], [("ff", r)])
            fw.op("dve", lambda e, r=r: e.scalar_tensor_tensor(pre[r][:], h1[r][:], ALPHA, ff[r][:], ALU.mult, ALU.add), [("h1", r), ("ff", r)], [("pre", r)])
            ln_rows(fw, T, pre[r][:], ("pre", r), lng[:], lnb[:], oo[r][:], ("oo", r), (sq,) + tuple(small))
            fw.dma(out[rows, :], oo[r][:], [("oo", r)], [("out", t)])
        fw.emit()

QK_M=512; V_M=1024; NH_M=4; Q_H=1024; V_H=1024
OFF={}
_o=0
for name,sz in [("qk",1024),("v",1024),("o",1024),("ig",8),("fg",8),("qh",1024),("fh",2048),("ih",1024),("gh",1024),("gm",2048),("gH",2048)]:
    OFF[name]=_o; _o+=sz

def cols_for(hg):
    r=np.arange
    fm=np.concatenate([OFF["qk"]+hg*128+r(128), OFF["qk"]+512+hg*128+r(128), OFF["qh"]+hg*256+r(256)])
    tm=np.concatenate([OFF["v"]+hg*256+r(256), OFF["o"]+hg*256+r(256), OFF["ih"]+hg*256+r(256), OFF["gh"]+hg*256+r(256),
                       OFF["fh"]+hg*256+r(256), OFF["fh"]+1024+hg*256+r(256),
                       [OFF["ig"]+hg, OFF["ig"]+4+hg, OFF["fg"]+hg, OFF["fg"]+4+hg]]).astype(np.int64)
    return fm,tm

def prep_A(inp, core, consts):
    b=core//4; hg=core%4
    fm,tm=cols_for(hg)
    w=inp["w_in"][0]; bi=inp["b_in"][0]
    d={}
    d["xb"]=np.ascontiguousarray(inp["x"][b])
    d["wfm"]=np.ascontiguousarray(w[:,fm]); d["wtm"]=np.ascontiguousarray(w[:,tm])
    d["bfm"]=np.ascontiguousarray(bi[fm].reshape(4,128).T.reshape(128,4,1))
    d["btm"]=np.ascontiguousarray(np.broadcast_to(bi[tm][None,:],(128,1540)))
    d["ln_g"]=np.ascontiguousarray(np.broadcast_to(inp["ln_in_g"][None,:],(128,2048)))
    d["ln_b"]=np.ascontiguousarray(np.broadcast_to(inp["ln_in_b"][None,:],(128,2048)))
    cw=inp["conv_w"][0]
    d["cw"]=np.ascontiguousarray(np.stack([cw[:,hg*128:(hg+1)*128].T, cw[:,512+hg*128:512+(hg+1)*128].T],1))
    d["gn_m"]=np.ascontiguousarray(np.broadcast_to(inp["mlstm_norm_g"][0,hg][None,:],(128,256)))
    d["gn_h"]=np.ascontiguousarray(np.broadcast_to(inp["hgrn_norm_g"][0,2*hg:2*hg+2].reshape(1,256),(128,256)))
    lg=inp["hgrn_lb_logits"]
    sel=lambda slot: np.concatenate([lg[dd,slot,hg*256:(hg+1)*256] for dd in range(2)])
    d["lb0"]=np.ascontiguousarray(np.broadcast_to(sel(0)[None,:],(128,512)))
    d["lb1"]=np.ascontiguousarray(np.broadcast_to(sel(1)[None,:],(128,512)))
    for k,v in consts.items(): d["c_"+k]=v
    return d

def prep_B(inp, core):
    b=core//4; q=core%4
    w=inp["w_in"][0]; bi=inp["b_in"][0]
    d={}
    d["xq"]=np.ascontiguousarray(inp["x"][b, q*2048:(q+1)*2048])
    d["w_g"]=np.ascontiguousarray(w[:, OFF["gm"]:OFF["gm"]+4096])
    d["b_g"]=np.ascontiguousarray(np.broadcast_to(bi[OFF["gm"]:OFF["gm"]+4096][None,:],(128,4096)))
    d["w_bm"]=np.ascontiguousarray(inp["w_branch_m"][0]); d["w_bh"]=np.ascontiguousarray(inp["w_branch_h"][0])
    d["w_out"]=np.ascontiguousarray(inp["w_out"][0])
    d["ln1_g"]=np.ascontiguousarray(np.broadcast_to(inp["ln1_g"][0][None,:],(128,2048)))
    d["ln1_b"]=np.ascontiguousarray(np.broadcast_to(inp["ln1_b"][0][None,:],(128,2048)))
    d["w_router"]=np.ascontiguousarray(inp["w_router"][0])
    p=np.arange(128)[:,None,None]; t=np.arange(16)[None,:,None]; hg=np.arange(4)[None,None,:]
    d["gidx"]=((4*b+hg)*8192 + q*2048 + t*128 + p).astype(np.int32)
    return d

def prep_CD(inp, core):
    b=core//4; q=core%4
    d={}
    sel=np.zeros((128,2,1,16),np.float32)
    for el in range(2): sel[:,el,0,2*core+el]=1.0
    d["selm"]=sel
    d["w_gate"]=np.ascontiguousarray(inp["w_gate_e"][0,2*core:2*core+2])
    d["w_up"]=np.ascontiguousarray(inp["w_up_e"][0,2*core:2*core+2])
    d["w_down"]=np.ascontiguousarray(inp["w_down_e"][0,2*core:2*core+2])
    p=np.arange(128)[:,None]
    g=np.arange(128)[None,:]
    r=g//16; el=(g%16)//8; jb=g%8
    d["drow"]=(r*4096+(b*2+el)*1024+jb*128+p).astype(np.int32)
    d["basef"]=np.full((128,1),b*8192+q*2048,np.float32)
    d["trash"]=(2048+np.arange(128,dtype=np.float32)).reshape(128,1)
    d["ln2_g"]=np.ascontiguousarray(np.broadcast_to(inp["ln2_g"][0][None,:],(128,2048)))
    d["ln2_b"]=np.ascontiguousarray(np.broadcast_to(inp["ln2_b"][0][None,:],(128,2048)))
    return d

def build_full():
    nc = bass.Bass("TRN2", target_bir_lowering=False)
    io={}
    def inp(name,shape,dt=F32):
        io[name]=nc.dram_tensor(name,shape,dt,kind="ExternalInput").ap()
    inp("xb",[8192,2048]); inp("wfm",[2048,512]); inp("wtm",[2048,1540]); inp("bfm",[128,4,1]); inp("btm",[128,1540])
    inp("ln_g",[128,2048]); inp("ln_b",[128,2048]); inp("cw",[128,2,5]); inp("gn_m",[128,256]); inp("gn_h",[128,256])
    inp("lb0",[128,512]); inp("lb1",[128,512])
    inp("xq",[2048,2048]); inp("w_g",[2048,4096]); inp("b_g",[128,4096]); inp("w_bm",[1024,2048]); inp("w_bh",[1024,2048]); inp("w_out",[2048,2048])
    inp("ln1_g",[128,2048]); inp("ln1_b",[128,2048]); inp("w_router",[2048,16]); inp("gidx",[128,16,4],I32)
    inp("selm",[128,2,1,16]); inp("w_gate",[2,2048,5632]); inp("w_up",[2,2048,5632]); inp("w_down",[2,5632,2048])
    inp("drow",[128,128],I32); inp("basef",[128,1]); inp("trash",[128,1]); inp("ln2_g",[128,2048]); inp("ln2_b",[128,2048])
    io["consts"]={}
    shapes=dict(CONST_SHAPES); shapes.update(C_CONST_SHAPES)
    for k,sh in shapes.items():
        io["consts"][k]=nc.dram_tensor("c_"+k,sh,F32,kind="ExternalInput").ap()
    hg=nc.dram_tensor("hg_out",[8192,512],BF16); io["hg_out"]=hg
    ag1=nc.dram_tensor("ag1",[65536,512],BF16); io["ag1"]=ag1.ap()
    io["d_h1"]=nc.dram_tensor("d_h1",[2048,2048],F32)
    h1b=nc.dram_tensor("h1b_out",[2048,2048],BF16); io["h1b_out"]=h1b
    affo=nc.dram_tensor("aff_out",[2048,16],F32); io["aff_out"]=affo
    ag2=nc.dram_tensor("ag2",[16384,2048],BF16); io["ag2"]=ag2.ap()
    agaff=nc.dram_tensor("ag_aff",[16384,16],F32); io["ag_aff"]=agaff.ap()
    yeo=nc.dram_tensor("ye_out",[4096,2048],F32); io["ye_out"]=yeo
    idxo=nc.dram_tensor("idx_out",[4096,1],I32); io["idx_out"]=idxo
    ag3=nc.dram_tensor("ag3",[32768,2048],F32); io["ag3"]=ag3.ap()
    agidx=nc.dram_tensor("ag_idx",[32768,1],I32); io["ag_idx"]=agidx.ap()
    io["out"]=nc.dram_tensor("out",[2048,2048],F32,kind="ExternalOutput")
    fw=FW(nc)
    rg=[list(range(8))]
    build_phase_a(nc,fw,io)
    keys=[("hg_out_m",n) for n in range(64)]+[("hg_out_h",n) for n in range(64)]
    fw.op("pool",lambda e:e.collective_compute("AllGather",ALU.bypass,replica_groups=rg,ins=[hg.ap().opt()],outs=[ag1.ap().opt()]),keys,["ag1"],kind="cc")
    fw.emit()
    build_phase_b(nc,fw,io)
    fw.op("pool",lambda e:e.collective_compute("AllGather",ALU.bypass,replica_groups=rg,ins=[h1b.ap().opt()],outs=[ag2.ap().opt()]),[("h1b_out",t) for t in range(16)],["ag2"],kind="cc")
    fw.op("pool",lambda e:e.collective_compute("AllGather",ALU.bypass,replica_groups=rg,ins=[affo.ap().opt()],outs=[agaff.ap().opt()]),[("aff_out",t) for t in range(16)],["ag_aff"],kind="cc")
    fw.emit()
    build_phase_c(nc,fw,io)
    yk=[("ye_out",L,jb,dmb) for L in range(4) for jb in range(8) for dmb in range(8)]
    fw.op("pool",lambda e:e.collective_compute("AllGather",ALU.bypass,replica_groups=rg,ins=[yeo.ap().opt()],outs=[ag3.ap().opt()]),yk,["ag3"],kind="cc")
    fw.op("pool",lambda e:e.collective_compute("AllGather",ALU.bypass,replica_groups=rg,ins=[idxo.ap().opt()],outs=[agidx.ap().opt()]),[("idx_out",L) for L in range(4)],["ag_idx"],kind="cc")
    fw.emit()
    build_phase_d(nc,fw,io)
    fw.final_wait()
    fw.close()
    return nc

def make_maps(inp):
    consts=host_consts(); consts.update(host_consts_c())
    maps=[]
    for c in range(8):
        d=prep_A(inp,c,consts); d.update(prep_B(inp,c)); d.update(prep_CD(inp,c)); maps.append(d)
    return maps


_NC = None


def kernel(**inputs):
    global _NC
    inp = {k: np.asarray(v) for k, v in inputs.items()}
    if _NC is None:
        _NC = build_full()
    maps = make_maps(inp)
    res = run_bass_kernel_spmd(_NC, maps, core_ids=list(range(8)))
    out = np.stack([np.asarray(r["out"]) for r in res.results]).reshape(2, 8192, 2048)
    return out.astype(np.float32)
```

```python
import numpy as np
from contextlib import ExitStack
import concourse.bass as bass
import concourse.mybir as mybir
from concourse.bass_utils import run_bass_kernel_spmd

F32 = mybir.dt.float32
BF16 = mybir.dt.bfloat16
I32 = mybir.dt.int32
ALU = mybir.AluOpType
AF = mybir.ActivationFunctionType
AX = mybir.AxisListType

SEM_LIMIT = 30000


class _Op:
    __slots__ = ("eng", "fn", "deps", "kind", "sig", "idx", "skip_pe")

    def __init__(self, eng, fn, kind):
        self.eng = eng
        self.fn = fn
        self.kind = kind
        self.deps = []
        self.sig = None
        self.idx = -1


class FW:
    ENG = ("pe", "act", "dve", "pool", "sp")

    def __init__(self, nc):
        self.nc = nc
        self.ops = []
        self.track = {}
        self.semctx = []
        self.cur = {}
        self.waited = {}
        self.all_sems = []
        self.last_sig = {}
        self.dq = {}
        self.prewait = {}

    def _newsem(self, name):
        self.nsem = getattr(self, "nsem", 0) + 1
        name = "%s_u%d" % (name, self.nsem)
        cm = self.nc.semaphore(name)
        s = cm.__enter__()
        self.semctx.append(cm)
        return s

    def close(self):
        for cm in reversed(self.semctx):
            cm.__exit__(None, None, None)
        self.semctx = []

    def op(self, eng, fn, reads=(), writes=(), kind="c"):
        o = _Op(eng, fn, kind)
        o.idx = len(self.ops)
        deps = set()
        for k in reads:
            t = self.track.get(k)
            if t is None:
                t = [None, []]
                self.track[k] = t
            if t[0] is not None:
                deps.add(t[0])
            t[1].append(o)
        for k in writes:
            t = self.track.get(k)
            if t is None:
                t = [None, []]
                self.track[k] = t
            if t[0] is not None:
                deps.add(t[0])
            for r in t[1]:
                if r is not o:
                    deps.add(r)
            t[0] = o
            t[1] = []
        deps.discard(o)
        o.deps = [d for d in deps]
        self.ops.append(o)
        return o

    def dma(self, out, in_, reads, writes, eng="sp", **kw):
        return self.op(eng, lambda e: e.dma_start(out=out, in_=in_, **kw), reads, writes, kind="d")

    def emit(self, barrier_first=True):
        nc = self.nc
        ops = self.ops
        prev_barrier = dict(self.last_sig) if barrier_first else {}
        src = set()
        for o in ops:
            for d in o.deps:
                if d.sig is None:
                    if d.eng == "pe" and o.eng == "pe" and d.kind == "c" and o.kind == "c":
                        continue
                    src.add(d)
        last = {}
        for o in ops:
            last[(o.eng, o.kind)] = o
        for o in last.values():
            src.add(o)
        for o in ops:
            if o.kind != "c":
                src.add(o)
        K = 16
        for o in ops:
            if o in src:
                key = (o.eng, o.kind)
                if o.kind == "cc":
                    s = self._newsem("cc%d" % o.idx)
                    o.sig = (s, 1)
                    continue
                if o.kind == "d":
                    q = self.dq.setdefault(o.eng, {"n": 0, "slots": [None] * K})
                    j = q["n"] % K
                    q["n"] += 1
                    c = q["slots"][j]
                    pre = None
                    if c is not None and c[1] > 0:
                        pre = (c[0], c[1])
                    if c is None or c[1] + 16 > SEM_LIMIT:
                        c = [self._newsem("d_%s_%d_%d" % (o.eng, j, o.idx)), 0]
                        q["slots"][j] = c
                    c[1] += 16
                    o.sig = (c[0], c[1])
                    self.prewait[o] = pre
                    self.last_sig[(o.eng, "d", j)] = o.sig
                    continue
                c = self.cur.get(key)
                if c is None or c[1] + 1 > SEM_LIMIT:
                    c = [self._newsem("s_%s_%s_%d" % (o.eng, o.kind, o.idx)), 0]
                    self.cur[key] = c
                c[1] += 1
                o.sig = (c[0], c[1])
        for k, o in last.items():
            if o.kind != "d":
                self.last_sig[k] = o.sig
        engmap = {"pe": "tensor", "act": "scalar", "dve": "vector", "pool": "gpsimd", "sp": "sync"}
        by_eng = {e: [o for o in ops if o.eng == e] for e in self.ENG}
        waited = self.waited

        def run_engine(ename, e):
            first = True
            for o in by_eng[ename]:
                need = {}
                if first:
                    first = False
                    for bk, sg in prev_barrier.items():
                        if sg is not None:
                            cur = need.get(id(sg[0]))
                            if cur is None or cur[1] < sg[1]:
                                need[id(sg[0])] = (sg[0], sg[1])
                pre = self.prewait.pop(o, None)
                if pre is not None:
                    cur = need.get(id(pre[0]))
                    if cur is None or cur[1] < pre[1]:
                        need[id(pre[0])] = pre
                for d in o.deps:
                    if d.sig is None:
                        continue
                    if d.eng == "pe" and o.eng == "pe" and d.kind == "c" and o.kind == "c":
                        continue
                    s, v = d.sig
                    cur = need.get(id(s))
                    if cur is None or cur[1] < v:
                        need[id(s)] = (s, v)
                for sid, (s, v) in need.items():
                    wk = (ename, sid)
                    if waited.get(wk, 0) >= v:
                        continue
                    waited[wk] = v
                    e.wait_ge(s, v)
                ins = o.fn(e)
                if o.sig is not None:
                    if o.kind == "d":
                        ins.then_inc(o.sig[0], 16)
                    elif o.kind == "cc":
                        ins.then_inc(o.sig[0])
                    else:
                        ins.then_inc(o.sig[0], 1)

        with nc.Block() as block:
            @block.tensor
            def _(e):
                run_engine("pe", e)

            @block.scalar
            def _(e):
                run_engine("act", e)

            @block.vector
            def _(e):
                run_engine("dve", e)

            @block.gpsimd
            def _(e):
                run_engine("pool", e)

            @block.sync
            def _(e):
                run_engine("sp", e)
        self.ops = []

    def final_wait(self, eng_name="pool"):
        nc = self.nc
        sigs = [sg for sg in self.last_sig.values() if sg is not None]
        with nc.Block() as block:
            @block.gpsimd
            def _(e):
                for s, v in sigs:
                    e.wait_ge(s, v)


S = 8192
D = 2048
NT = S // 128
EPS = 1e-5
ALPHA = 2.0 ** 0.25


def host_consts():
    s = np.arange(128)[:, None]
    t = np.arange(128)[None, :]
    c = {}
    c["ident"] = np.eye(128, dtype=np.float32)
    c["trif"] = (s <= t).astype(np.float32)
    c["trib"] = (s >= t).astype(np.float32)
    c["ones"] = np.ones((128, 128), np.float32)
    same = (s // 64) == (t // 64)
    c["hmaskf"] = (same & (s <= t)).astype(np.float32)
    c["hmaskb"] = (same & (s >= t)).astype(np.float32)
    midf = (t // 64) * 64 + 31
    midb = (t // 64) * 64 + 32
    c["Mf"] = (same * ((s <= t).astype(np.float32) - (s <= midf).astype(np.float32))).astype(np.float32)
    c["Mb"] = (same * ((s >= t).astype(np.float32) - (s >= midb).astype(np.float32))).astype(np.float32)
    xf = np.zeros((128, 6), np.float32)
    xb = np.zeros((128, 6), np.float32)
    sv = np.arange(128)
    for ch in range(2):
        inch = (sv // 64) == ch
        mf = ch * 64 + 31
        mb = ch * 64 + 32
        xf[:, 3 * ch + 0] = inch & (sv <= mf)
        xf[:, 3 * ch + 1] = inch
        xf[:, 3 * ch + 2] = inch & (sv > mf)
        xb[:, 3 * ch + 0] = inch & (sv >= mb)
        xb[:, 3 * ch + 1] = inch
        xb[:, 3 * ch + 2] = inch & (sv < mb)
    c["Mxf"] = np.concatenate([c["Mf"], xf], 1)
    c["Mxb"] = np.concatenate([c["Mb"], xb], 1)
    return c


CONST_SHAPES = {"ident": [128, 128], "trif": [128, 128], "trib": [128, 128], "ones": [128, 128],
                "hmaskf": [128, 128], "hmaskb": [128, 128], "Mf": [128, 128], "Mb": [128, 128],
                "Mxf": [128, 134], "Mxb": [128, 134]}


def ln_rows(fw, T, x_t, key, g_t, b_t, out_t, okey, tmp):
    sq, ssum, ssq, mean, var, rstd, nb = tmp
    k = key
    fw.op("dve", lambda e: e.reduce_sum(ssum[:], x_t, AX.X), [k], ["ln_ssum"])
    fw.op("act", lambda e: e.activation(sq[:], x_t, AF.Square), [k], ["ln_sq"])
    fw.op("dve", lambda e: e.reduce_sum(ssq[:], sq[:], AX.X), ["ln_sq"], ["ln_ssq"])
    fw.op("dve", lambda e: e.tensor_scalar(mean[:], ssum[:], 1.0 / D, None, ALU.mult), ["ln_ssum"], ["ln_mean"])
    fw.op("dve", lambda e: e.tensor_tensor(var[:], mean[:], mean[:], ALU.mult), ["ln_mean"], ["ln_var"])
    fw.op("dve", lambda e: e.scalar_tensor_tensor(var[:], ssq[:], 1.0 / D, var[:], ALU.mult, ALU.subtract), ["ln_ssq", "ln_var"], ["ln_var"])
    fw.op("dve", lambda e: e.tensor_scalar(var[:], var[:], EPS, None, ALU.add), ["ln_var"], ["ln_var"])
    fw.op("act", lambda e: e.activation(rstd[:], var[:], AF.Sqrt), ["ln_var"], ["ln_rstd"])
    fw.op("dve", lambda e: e.reciprocal(rstd[:], rstd[:]), ["ln_rstd"], ["ln_rstd"])
    fw.op("dve", lambda e: e.scalar_tensor_tensor(nb[:], mean[:], -1.0, rstd[:], ALU.mult, ALU.mult), ["ln_mean", "ln_rstd"], ["ln_nb"])
    fw.op("act", lambda e: e.activation(sq[:], x_t, AF.Identity, bias=nb[:], scale=rstd[:]), [k, "ln_nb", "ln_rstd", "ln_sq"], ["ln_sq"])
    fw.op("pool", lambda e: e.tensor_tensor(sq[:], sq[:], g_t, ALU.mult), ["ln_sq", "lng"], ["ln_sq"])
    fw.op("dve", lambda e: e.tensor_tensor(out_t, sq[:], b_t, ALU.add), ["ln_sq", "lnb"], [okey])


def build_phase_a(nc, fw, io, dbg=False):
    xb = io["xb"]
    d_fm = nc.dram_tensor("d_fm", [512, S + 4], F32)
    d_tm = nc.dram_tensor("d_tm", [S, 1540], F32)
    d_hm = nc.dram_tensor("d_hm", [S, 256], F32)
    d_ho = nc.dram_tensor("d_ho", [S, 256], F32)
    hg_out = io["hg_out"]
    C = io["consts"]

    with ExitStack() as st:
        T = lambda n, sh, dt: st.enter_context(nc.sbuf_tensor("s_" + n, sh, dt))
        P = lambda n, sh, dt: st.enter_context(nc.psum_tensor("p_" + n, sh, dt))
        wfm = T("wfm", [128, 16, 512], BF16)
        wtm = T("wtm", [128, 16, 1540], BF16)
        lng = T("lng", [128, D], F32)
        lnb = T("lnb", [128, D], F32)
        bfm = T("bfm", [128, 4, 1], F32)
        btm = T("btm", [128, 1540], F32)
        identb = T("identb", [128, 128], BF16)
        zero = T("zero", [128, 4, 2], F32)
        xt = [T("xt%d" % i, [128, D], F32) for i in range(2)]
        sq = T("sq", [128, D], F32)
        hb = [T("hb%d" % i, [128, D], BF16) for i in range(2)]
        hT = [T("hT%d" % i, [128, 16, 128], BF16) for i in range(2)]
        fmo = [T("fmo%d" % i, [128, 4, 128], F32) for i in range(2)]
        tmo = [T("tmo%d" % i, [128, 1540], F32) for i in range(2)]
        small = [T("sm%d" % i, [128, 1], F32) for i in range(6)]
        pT = [P("pT%d" % i, [128, 8, 128], BF16) for i in range(2)]
        pfm = P("pfm", [128, 4, 128], F32)
        ptm = [P("ptm%d" % i, [128, 512], F32) for i in range(3)]
        pg = P("pg", [128, 4], F32)
        for k in range(16):
            fw.dma(wfm[:, k, :], io["wfm"][k * 128:(k + 1) * 128, :], [], [("wfm", k)], eng="pool")
            fw.dma(wtm[:, k, :], io["wtm"][k * 128:(k + 1) * 128, :], [], [("wtm", k)], eng="pool")
        fw.dma(lng[:], io["ln_g"], [], ["lng"])
        fw.dma(lnb[:], io["ln_b"], [], ["lnb"])
        fw.dma(bfm[:], io["bfm"], [], ["bfm"])
        fw.dma(btm[:], io["btm"], [], ["btm"])
        fw.dma(identb[:], C["ident"], [], ["identb"], eng="pool")
        fw.op("dve", lambda e: e.memset(zero[:], 0.0), [], ["zero"])
        fw.dma(d_fm[:, 0:2].rearrange("(b p) t -> p b t", p=128), zero[:], ["zero"], ["d_fm_h0"])
        fw.dma(d_fm[:, S + 2:S + 4].rearrange("(b p) t -> p b t", p=128), zero[:], ["zero"], ["d_fm_h1"])
        def stage_ln(n):
            x_ = xt[n % 2]
            fw.dma(x_[:], xb[n * 128:(n + 1) * 128, :], [], [("xt", n % 2)])
            ln_rows(fw, T, x_[:], ("xt", n % 2), lng[:], lnb[:], hb[n % 2][:], ("hb", n % 2), (sq,) + tuple(small))

        def stage_mm(n):
            hT_ = hT[n % 2]
            hb_ = hb[n % 2]
            for half in range(2):
                pt = pT[half]
                for j in range(8):
                    k = half * 8 + j
                    fw.op("pe", lambda e, pt=pt, j=j, k=k: e.transpose(pt[:, j, :], hb_[:, k * 128:(k + 1) * 128], identb[:]), [("hb", n % 2), "identb"], [("pT", half)])
                if half == 0:
                    fw.op("act", lambda e, pt=pt, hT_=hT_: e.copy(hT_[:, 0:8, :], pt[:]), [("pT", 0)], [("hT", n % 2, 0)])
                else:
                    fw.op("dve", lambda e, pt=pt, hT_=hT_: e.tensor_copy(hT_[:, 8:16, :], pt[:]), [("pT", 1)], [("hT", n % 2, 1)])
            hk = [("hT", n % 2, 0), ("hT", n % 2, 1)]
            for blk in range(4):
                for k in range(16):
                    fw.op("pe", lambda e, blk=blk, k=k, hT_=hT_: e.matmul(pfm[:, blk, :], wfm[:, k, blk * 128:(blk + 1) * 128], hT_[:, k, :], start=(k == 0), stop=(k == 15)),
                          hk + [("wfm", k)], ["pfm"])
            fo = fmo[n % 2]
            fw.op("dve", lambda e, fo=fo: e.tensor_tensor(fo[:], pfm[:], bfm[:].to_broadcast([128, 4, 128]), ALU.add), ["pfm", "bfm"], [("fmo", n % 2)])
            fw.dma(d_fm[:, 2 + n * 128:2 + (n + 1) * 128].rearrange("(b p) t -> p b t", p=128), fo[:], [("fmo", n % 2)], [("d_fm", n)], eng="act")
            to = tmo[n % 2]
            for cb in range(3):
                for k in range(16):
                    fw.op("pe", lambda e, cb=cb, k=k, hT_=hT_: e.matmul(ptm[cb][:], hT_[:, k, :], wtm[:, k, cb * 512:(cb + 1) * 512], start=(k == 0), stop=(k == 15)),
                          hk + [("wtm", k)], [("ptm", cb)])
                eng = "dve" if cb != 1 else "pool"
                if eng == "pool":
                    fw.op("act", lambda e, cb=cb, to=to: e.copy(to[:, cb * 512:(cb + 1) * 512], ptm[cb][:]), [("ptm", cb)], [("tmo", n % 2, cb)])
                    fw.op("pool", lambda e, cb=cb, to=to: e.tensor_tensor(to[:, cb * 512:(cb + 1) * 512], to[:, cb * 512:(cb + 1) * 512], btm[:, cb * 512:(cb + 1) * 512], ALU.add), [("tmo", n % 2, cb), "btm"], [("tmo", n % 2, cb)])
                else:
                    fw.op("dve", lambda e, cb=cb, to=to: e.tensor_tensor(to[:, cb * 512:(cb + 1) * 512], ptm[cb][:], btm[:, cb * 512:(cb + 1) * 512], ALU.add), [("ptm", cb), "btm"], [("tmo", n % 2, cb)])
            for k in range(16):
                fw.op("pe", lambda e, k=k, hT_=hT_: e.matmul(pg[:], hT_[:, k, :], wtm[:, k, 1536:1540], start=(k == 0), stop=(k == 15)), hk + [("wtm", k)], ["pg"])
            fw.op("dve", lambda e, to=to: e.tensor_tensor(to[:, 1536:1540], pg[:], btm[:, 1536:1540], ALU.add), ["pg", "btm"], [("tmo", n % 2, 3)])
            fw.dma(d_tm[n * 128:(n + 1) * 128, :], to[:], [("tmo", n % 2, i) for i in range(4)], [("d_tm", n)], eng="act")

        stage_ln(0)
        for n in range(NT):
            if n + 1 < NT:
                stage_ln(n + 1)
            stage_mm(n)
        fw.emit()

    with ExitStack() as st:
        T = lambda n, sh, dt: st.enter_context(nc.sbuf_tensor("s_" + n, sh, dt))
        P = lambda n, sh, dt: st.enter_context(nc.psum_tensor("p_" + n, sh, dt))
        cn = {}
        for name in ["ident", "trif", "trib", "ones", "hmaskf", "hmaskb", "Mf", "Mb", "Mxf", "Mxb"]:
            cn[name] = T("c_" + name, CONST_SHAPES[name], F32)
            fw.dma(cn[name][:], C[name], [], ["c_" + name])
        identb = T("identb2", [128, 128], BF16)
        fw.dma(identb[:], C["ident"], [], ["identb"], eng="pool")
        cw = T("cw", [128, 2, 5], F32)
        fw.dma(cw[:], io["cw"], [], ["cw"])
        gnm = T("gnm", [128, 256], F32)
        gnh = T("gnh", [128, 256], F32)
        fw.dma(gnm[:], io["gn_m"], [], ["gnm"])
        fw.dma(gnh[:], io["gn_h"], [], ["gnh"])
        lb = T("lb", [128, 512], F32)
        oml = T("oml", [128, 512], F32)
        l1 = T("l1", [128, 512], F32)
        fw.dma(lb[:], io["lb0"], [], ["lb"])
        fw.dma(l1[:], io["lb1"], [], ["l1"])
        fw.op("dve", lambda e: e.tensor_tensor(lb[:], lb[:], l1[:], ALU.subtract), ["lb", "l1"], ["lb"])
        fw.op("act", lambda e: e.activation(lb[:], lb[:], AF.Sigmoid), ["lb"], ["lb"])
        fw.op("dve", lambda e: e.tensor_scalar(oml[:], lb[:], -1.0, 1.0, ALU.mult, ALU.add), ["lb"], ["oml"])

        gates = T("gates", [128, NT, 4], F32)
        for n0 in range(0, NT, 8):
            fw.dma(gates[:, n0:n0 + 8, :], d_tm[n0 * 128:(n0 + 8) * 128, 1536:1540].rearrange("(n p) c -> p n c", p=128), [("d_tm", n) for n in range(n0, n0 + 8)], ["gates"])
        lf = T("lf", [128, 2, NT], F32)
        li = T("li", [128, 2, NT], F32)
        zz = T("zz", [128, 2, NT], F32)
        egl = T("egl", [128, 2, NT], F32)
        Zs = T("Zs", [128, 2, NT], F32)
        Ee = T("Ee", [128, 2, NT], F32)
        aa = T("aa", [128, 2, NT], F32)
        sc = T("sc", [128, 2, NT], F32)
        thr = T("thr", [128, 2, NT], F32)
        gcs = T("gcs", [128, 2, NT], F32)
        pst = ExitStack()
        PP = lambda n, sh, dt: pst.enter_context(nc.psum_tensor("pp_" + n, sh, dt))
        pA = PP("pA", [128, 2, NT], F32)
        pB = PP("pB", [128, 2, NT], F32)
        for d in range(2):
            fw.op("act", lambda e, d=d: e.activation(lf[:, d, :], gates[:, :, 2 + d], AF.Exp, scale=-1.0), ["gates"], [("lf", d)])
            fw.op("dve", lambda e, d=d: e.tensor_copy(li[:, d, :], gates[:, :, d]), ["gates"], [("li", d)])
        for d in range(2):
            fw.op("act", lambda e, d=d: e.activation(lf[:, d, :], lf[:, d, :], AF.Ln, bias=1.0), [("lf", d)], [("lf", d)])
            fw.op("dve", lambda e, d=d: e.tensor_scalar(lf[:, d, :], lf[:, d, :], -1.0, None, ALU.mult), [("lf", d)], [("lf", d)])
        tri = [cn["trif"], cn["trib"]]
        for d in range(2):
            fw.op("pe", lambda e, d=d: e.matmul(pA[:, d, :], tri[d][:], lf[:, d, :], start=True, stop=True), [("lf", d), "c_trif", "c_trib"], [("pA", d)])
            fw.op("pe", lambda e, d=d: e.matmul(pB[:, d, :], cn["ones"][:], lf[:, d, :], start=True, stop=True), [("lf", d), "c_ones"], [("pB", d)])
        fw.op("dve", lambda e: e.tensor_copy(gcs[:], pA[:]), [("pA", 0), ("pA", 1)], ["gcs"])
        fw.op("act", lambda e: e.activation(egl[:], pB[:], AF.Exp), [("pB", 0), ("pB", 1)], ["egl"])
        fw.op("dve", lambda e: e.tensor_tensor(zz[:], li[:], gcs[:], ALU.subtract), [("li", 0), ("li", 1), "gcs"], ["zz"])
        fw.op("act", lambda e: e.activation(zz[:], zz[:], AF.Exp), ["zz"], ["zz"])
        fw.op("act", lambda e: e.activation(thr[:], gcs[:], AF.Exp, scale=-1.0), ["gcs"], ["thr"])
        for d in range(2):
            fw.op("pe", lambda e, d=d: e.matmul(pA[:, d, :], cn["ones"][:], zz[:, d, :], start=True, stop=True), ["zz", "c_ones", "gcs"], [("pA", d)])
        fw.op("dve", lambda e: e.tensor_copy(Zs[:], pA[:]), [("pA", 0), ("pA", 1)], ["Zs"])
        fw.op("dve", lambda e: e.memset(Ee[:, 0, 0:1], 1.0), [], ["Ee"])
        fw.op("dve", lambda e: e.memset(Ee[:, 1, NT - 1:NT], 1.0), ["Ee"], ["Ee"])
        for n in range(NT - 1):
            fw.op("dve", lambda e, n=n: e.scalar_tensor_tensor(Ee[:, 0, n + 1:n + 2], Ee[:, 0, n:n + 1], Zs[:, 0, n:n + 1], egl[:, 0, n:n + 1], ALU.add, ALU.mult), ["Ee", "Zs", "egl"], ["Ee"])
            m = NT - 1 - n
            fw.op("dve", lambda e, m=m: e.scalar_tensor_tensor(Ee[:, 1, m - 1:m], Ee[:, 1, m:m + 1], Zs[:, 1, m:m + 1], egl[:, 1, m:m + 1], ALU.add, ALU.mult), ["Ee", "Zs", "egl"], ["Ee"])
        fw.op("dve", lambda e: e.tensor_tensor(Zs[:], Zs[:], Ee[:], ALU.add), ["Zs", "Ee"], ["Zs"])
        fw.op("dve", lambda e: e.reciprocal(Zs[:], Zs[:]), ["Zs"], ["Zs"])
        fw.op("dve", lambda e: e.tensor_tensor(sc[:], Ee[:], Zs[:], ALU.mult), ["Zs", "Ee"], ["sc"])
        fw.op("dve", lambda e: e.tensor_tensor(thr[:], thr[:], Zs[:], ALU.mult), ["Zs", "thr"], ["thr"])
        fw.op("dve", lambda e: e.scalar_tensor_tensor(aa[:], zz[:], 128.0 ** -0.5, Zs[:], ALU.mult, ALU.mult), ["Zs", "zz"], ["aa"])

        qT = T("qT", [128, NT, 128], BF16)
        kT = T("kT", [128, NT, 128], BF16)
        ktm = T("ktm", [128, NT, 128], BF16)
        vall = T("vall", [128, NT, 257], BF16)
        fw.op("pool", lambda e: e.memset(vall[:, :, 256:257], 1.0), [], ["vones"])
        for n0 in range(0, NT, 8):
            fw.dma(vall[:, n0:n0 + 8, 0:256], d_tm[n0 * 128:(n0 + 8) * 128, 0:256].rearrange("(n p) c -> p n c", p=128),
                   [("d_tm", n) for n in range(n0, n0 + 8)], [("vall", n0)], eng="pool")
        cin = [T("cin%d" % i, [128, 2, 132], F32) for i in range(2)]
        cacc = [T("cacc%d" % i, [128, 2, 128], F32) for i in range(2)]
        ctmp = T("ctmp", [128, 128], F32)
        pk = [PP("pk%d" % i, [128, 128], BF16) for i in range(2)]
        for n in range(NT):
            ci = cin[n % 2]
            ca = cacc[n % 2]
            fw.dma(ci[:], d_fm[0:256, n * 128:n * 128 + 132].rearrange("(b p) t -> p b t", p=128),
                   [("d_fm", m) for m in range(max(0, n - 1), min(NT, n + 2))] + ["d_fm_h0", "d_fm_h1"], [("cin", n % 2)])
            for qk, eng in ((0, "dve"), (1, "dve")):
                fw.op(eng, lambda e, qk=qk, ci=ci, ca=ca: e.tensor_scalar(ca[:, qk, :], ci[:, qk, 0:128], cw[:, qk, 0:1], None, ALU.mult), [("cin", n % 2), "cw"], [("cacc", n % 2, qk)])
                for j in range(1, 5):
                    if eng == "dve":
                        fw.op(eng, lambda e, qk=qk, ci=ci, ca=ca, j=j: e.scalar_tensor_tensor(ca[:, qk, :], ci[:, qk, j:j + 128], cw[:, qk, j:j + 1], ca[:, qk, :], ALU.mult, ALU.add),
                              [("cin", n % 2), "cw", ("cacc", n % 2, qk)], [("cacc", n % 2, qk)])
                    else:
                        fw.op(eng, lambda e, qk=qk, ci=ci, j=j: e.tensor_scalar(ctmp[:], ci[:, qk, j:j + 128], cw[:, qk, j:j + 1], None, ALU.mult), [("cin", n % 2), "cw"], ["ctmp"])
                        fw.op(eng, lambda e, qk=qk, ca=ca: e.tensor_tensor(ca[:, qk, :], ca[:, qk, :], ctmp[:], ALU.add), ["ctmp", ("cacc", n % 2, qk)], [("cacc", n % 2, qk)])
            fw.op("act", lambda e, ca=ca, n=n: e.activation(qT[:, n, :], ca[:, 0, :], AF.Silu), [("cacc", n % 2, 0)], [("qT", n)])
            fw.op("act", lambda e, ca=ca, n=n: e.activation(kT[:, n, :], ca[:, 1, :], AF.Silu), [("cacc", n % 2, 1)], [("kT", n)])
            fw.op("pe", lambda e, n=n: e.transpose(pk[n % 2][:], kT[:, n, :], identb[:]), [("kT", n), "identb"], [("pk", n % 2)])
            fw.op("act", lambda e, n=n: e.copy(ktm[:, n, :], pk[n % 2][:]), [("pk", n % 2)], [("ktm", n)])
        fw.emit()
        pst.close()

        Cst = [T("Cst%d" % d, [128, 257], F32) for d in range(2)]
        Sst = [[T("Sst%d_%d" % (d, h), [128, 128], F32) for h in range(2)] for d in range(2)]
        R2 = lambda name, sh, dt, cnt=2: [T("%s%d" % (name, i), sh, dt) for i in range(cnt)]
        WT = R2("WT", [128, 128], BF16)
        kp = R2("kp", [128, 128], BF16)
        Csc32 = R2("Csc32", [128, 257], F32)
        Csc16 = R2("Csc16", [128, 257], BF16)
        den = R2("den", [128, 1], F32)
        hmo = R2("hmo", [128, 256], F32)
        hfw = R2("hfw", [128, 256], F32)
        osig = R2("osig", [128, 256], F32)
        hsq = R2("hsq", [128, 256], F32)
        ssq = R2("ssq", [128, 1], F32)
        hgo = R2("hgo", [128, 256], BF16)
        fpre = R2("fpre", [128, 256], F32)
        ff = R2("ff", [128, 256], F32)
        logf = R2("logf", [128, 256], F32)
        kkk = R2("kkk", [128, 256], F32)
        eneg = R2("eneg", [128, 256], F32)
        ktl = R2("ktl", [128, 256], BF16)
        qin = R2("qin", [128, 2, 128], F32)
        itl = R2("itl", [128, 256], BF16)
        Eq = R2("Eq", [128, 128], F32, 4)
        ex = R2("ex", [128, 6], F32, 4)
        Qz = [T("Qz%d" % i, [128, 2, 128], BF16) for i in range(4)]
        ktT = R2("ktT", [128, 128], BF16, 4)
        AT = R2("AT", [128, 128], BF16, 4)
        Smid = R2("Smid", [128, 128], BF16, 4)
        hoo = R2("hoo", [128, 256], F32)
        hofw = R2("hofw", [128, 256], F32)
        gsl = R2("gsl", [128, 256], F32)
        hosq = R2("hosq", [128, 256], F32)
        hssq = R2("hssq", [128, 2], F32)
        hogo = R2("hogo", [128, 256], BF16)
        PS = []
        for dd_ in range(2):
            bA = P("bA%d" % dd_, [128, 512], F32)
            bB = P("bB%d" % dd_, [128, 512], F32)
            bC = P("bC%d" % dd_, [128, 512], F32)
            bD = P("bD%d" % dd_, [128, 512], F32)
            PS.append(dict(p_st=bA[:, 0:128], p_a=bA[:, 128:256], p_s=bA[:, 256:384], p_kt=bA[:, 384:512],
                           p_num=bB[:, 0:257], p_dc=bC[:, 0:257], p_x=bC[:, 257:391], p_b=bD[:, 0:256], p_o=bD[:, 256:512]))
        ktl32 = R2("ktl32", [128, 256], F32)
        identf = cn["ident"]
        d_hmb = nc.dram_tensor("d_hmb", [S, 256], F32)
        d_hob = nc.dram_tensor("d_hob", [S, 256], F32)
        for i in range(4):
            fw.op("pool", lambda e, i=i: e.memset(Qz[i][:], 0.0), [], [("Qz", i)])
        for d in range(2):
            fw.op("dve", lambda e, d=d: e.memset(Cst[d][:], 0.0), [], [("Cst", d)])
            for h in range(2):
                fw.op("dve", lambda e, d=d, h=h: e.memset(Sst[d][h][:], 0.0), [], [("Sst", d, h)])
        cnt = {"i": 0}

        def tile_body(d, it_):
            n = it_ if d == 0 else NT - 1 - it_
            mask_m = tri[d]
            hmask = cn["hmaskf"] if d == 0 else cn["hmaskb"]
            Mt = cn["Mf"] if d == 0 else cn["Mb"]
            Mx = cn["Mxf"] if d == 0 else cn["Mxb"]
            p_st = PS[d]["p_st"]; p_a = PS[d]["p_a"]; p_s = PS[d]["p_s"]; p_kt = PS[d]["p_kt"]
            p_num = PS[d]["p_num"]; p_dc = PS[d]["p_dc"]; p_x = PS[d]["p_x"]; p_b = PS[d]["p_b"]; p_o = PS[d]["p_o"]
            kd = lambda name, d=d: (name, d)
            r = d
            a_col = aa[:, d, n:n + 1]
            sc_col = sc[:, d, n:n + 1]
            th_col = thr[:, d, n:n + 1]
            fw.op("pe", lambda e, n=n: e.matmul(p_st, kT[:, n, :], qT[:, n, :], start=True, stop=True), [("kT", n), ("qT", n)], [("p_st", d)])
            yield
            fw.op("dve", lambda e, r=r, a_col=a_col, mask_m=mask_m: e.scalar_tensor_tensor(WT[r][:], p_st, a_col, mask_m[:], ALU.mult, ALU.mult), [("p_st", d), "aa", "c_trif", "c_trib"], [("WT", r)])
            yield
            fw.op("pool", lambda e, r=r, n=n, a_col=a_col: e.tensor_scalar(kp[r][:], ktm[:, n, :], a_col, None, ALU.mult), [("ktm", n), "aa"], [("kp", r)])
            yield
            fw.op("dve", lambda e, r=r, d=d, sc_col=sc_col: e.tensor_scalar(Csc32[r][:], Cst[d][:], sc_col, None, ALU.mult), [("Cst", d), "sc"], [("Csc32", r)])
            yield
            fw.op("act", lambda e, r=r: e.copy(Csc16[r][:], Csc32[r][:]), [("Csc32", r)], [("Csc16", r)])
            yield
            vk = [("vall", (n // 8) * 8), "vones"]
            fw.op("pe", lambda e, r=r, n=n: e.matmul(p_num, WT[r][:], vall[:, n, :], start=True, stop=False), [("WT", r)] + vk, [("p_num", d)])
            yield
            fw.op("pe", lambda e, r=r, n=n: e.matmul(p_num, qT[:, n, :], Csc16[r][:], start=False, stop=True), [("qT", n), ("Csc16", r)], [("p_num", d)])
            yield
            fw.op("pe", lambda e, r=r, n=n: e.matmul(p_dc, kp[r][:], vall[:, n, :], start=True, stop=True), [("kp", r)] + vk, [("p_dc", d)])
            yield
            fw.op("dve", lambda e, r=r, d=d: e.tensor_tensor(Cst[d][:], Csc32[r][:], p_dc, ALU.add), [("Csc32", r), ("p_dc", d)], [("Cst", d)])
            yield
            fw.op("act", lambda e, r=r: e.activation(den[r][:], p_num[:, 256:257], AF.Abs), [("p_num", d)], [("den", r)])
            yield
            fw.op("dve", lambda e, r=r, th_col=th_col: e.tensor_tensor(den[r][:], den[r][:], th_col, ALU.max), [("den", r), "thr"], [("den", r)])
            yield
            fw.op("dve", lambda e, r=r: e.reciprocal(den[r][:], den[r][:]), [("den", r)], [("den", r)])
            yield
            fw.op("act", lambda e, r=r: e.activation(hmo[r][:], p_num[:, 0:256], AF.Copy, scale=den[r][:]), [("p_num", d), ("den", r)], [("hmo", r)])
            yield
            fw.dma((d_hm if d == 0 else d_hmb)[n * 128:(n + 1) * 128, :], hmo[r][:], [("hmo", r)], [("d_hm", d, n)])
            yield
            fw.dma(fpre[r][:], d_tm[n * 128:(n + 1) * 128, 1024 + d * 256:1024 + (d + 1) * 256], [("d_tm", n)], [("fpre", r)])
            yield
            fw.dma(qin[r][:], d_fm[256:512, 2 + n * 128:2 + (n + 1) * 128].rearrange("(b p) t -> p b t", p=128), [("d_fm", n)], [("qin", r)])
            yield
            fw.dma(itl[r][:], d_tm[n * 128:(n + 1) * 128, 512:768], [("d_tm", n)], [("itl", r)], eng="pool")
            yield
            fw.op("act", lambda e, r=r: e.activation(ff[r][:], fpre[r][:], AF.Sigmoid), [("fpre", r)], [("ff", r)])
            yield
            fw.op("dve", lambda e, r=r, d=d: e.tensor_tensor(ff[r][:], ff[r][:], oml[:, d * 256:(d + 1) * 256], ALU.mult), [("ff", r), "oml"], [("ff", r)])
            yield
            fw.op("dve", lambda e, r=r, d=d: e.tensor_tensor(ff[r][:], ff[r][:], lb[:, d * 256:(d + 1) * 256], ALU.add), [("ff", r), "lb"], [("ff", r)])
            yield
            fw.op("act", lambda e, r=r: e.activation(logf[r][:], ff[r][:], AF.Ln), [("ff", r)], [("logf", r)])
            yield
            fw.op("pool", lambda e, r=r: e.tensor_scalar(kkk[r][:], ff[r][:], -1.0, 1.0, ALU.mult, ALU.add), [("ff", r)], [("kkk", r)])
            yield
            fw.op("pe", lambda e, r=r, Mt=Mt: e.matmul(p_b, Mt[:], logf[r][:], start=True, stop=True), [("logf", r), "c_Mf", "c_Mb"], [("p_b", d)])
            yield
            fw.op("act", lambda e, r=r: e.activation(eneg[r][:], p_b, AF.Exp, scale=-1.0), [("p_b", d)], [("eneg", r)])
            yield
            fw.op("dve", lambda e, r=r: e.tensor_tensor(ktl32[r][:], kkk[r][:], eneg[r][:], ALU.mult), [("kkk", r), ("eneg", r)], [("ktl32", r)])
            yield
            fw.op("pool", lambda e, r=r: e.tensor_copy(ktl[r][:], ktl32[r][:]), [("ktl32", r)], [("ktl", r)])
            yield
            for h in range(2):
                q = cnt.setdefault("q", 0) % 4
                cnt["q"] = cnt.get("q", 0) + 1
                hs = slice(h * 128, (h + 1) * 128)
                fw.op("pe", lambda e, r=r, hs=hs, Mx=Mx: e.matmul(p_x, logf[r][:, hs], Mx[:], start=True, stop=True), [("logf", r), "c_Mxf", "c_Mxb"], [("p_x", d)])
                yield
                fw.op("act", lambda e, q=q: e.activation(Eq[q][:], p_x[:, 0:128], AF.Exp), [("p_x", d)], [("Eq", q)])
                yield
                fw.op("act", lambda e, q=q: e.activation(ex[q][:], p_x[:, 128:134], AF.Exp), [("p_x", d)], [("ex", q)])
                yield
                fw.op("dve", lambda e, q=q, r=r, h=h: e.tensor_tensor(Qz[q][:, 0, 0:64], qin[r][:, h, 0:64], Eq[q][:, 0:64], ALU.mult), [("qin", r), ("Eq", q)], [("Qz", q)])
                yield
                fw.op("dve", lambda e, q=q, r=r, h=h: e.tensor_tensor(Qz[q][:, 1, 64:128], qin[r][:, h, 64:128], Eq[q][:, 64:128], ALU.mult), [("qin", r), ("Eq", q)], [("Qz", q)])
                yield
                fw.op("pe", lambda e, r=r, hs=hs: e.transpose(p_kt, ktl32[r][:, hs], identf[:]), [("ktl32", r), "c_ident"], [("p_kt", d)])
                yield
                fw.op("act", lambda e, q=q: e.copy(ktT[q][:], p_kt), [("p_kt", d)], [("ktT", q)])
                yield
                fw.op("pe", lambda e, q=q: e.matmul(p_a[:, 0:64], ktT[q][:], Qz[q][:, 0, 0:64], start=True, stop=True), [("ktT", q), ("Qz", q)], [("p_a", d)])
                yield
                fw.op("pe", lambda e, q=q: e.matmul(p_a[:, 64:128], ktT[q][:], Qz[q][:, 1, 64:128], start=True, stop=True), [("ktT", q), ("Qz", q)], [("p_a", d)])
                yield
                fw.op("dve", lambda e, q=q, hmask=hmask: e.tensor_tensor(AT[q][:], p_a, hmask[:], ALU.mult), [("p_a", d), "c_hmaskf", "c_hmaskb"], [("AT", q)])
                yield
                fw.op("pe", lambda e, q=q, r=r, hs=hs: e.matmul(p_o[:, hs], AT[q][:], itl[r][:, hs], start=True, stop=False), [("AT", q), ("itl", r)], [("p_o", d, h)])
                yield
                corder = (0, 1) if d == 0 else (1, 0)
                for ci_, c in enumerate(corder):
                    sm = cnt.setdefault("sm", 0) % 4
                    cnt["sm"] = cnt.get("sm", 0) + 1
                    fw.op("dve", lambda e, sm=sm, d=d, h=h, q=q, c=c: e.tensor_scalar(Smid[sm][:], Sst[d][h][:], ex[q][:, 3 * c:3 * c + 1], None, ALU.mult), [("Sst", d, h), ("ex", q)], [("Smid", sm)])
                    yield
                    fw.op("pe", lambda e, sm=sm, q=q, c=c, hs=hs, ci_=ci_: e.matmul(p_o[:, hs], Qz[q][:, c, :], Smid[sm][:], start=False, stop=(ci_ == 1)), [("Qz", q), ("Smid", sm)], [("p_o", d, h)])
                    yield
                    ps_ = slice(64 * c, 64 * c + 64)
                    fw.op("pe", lambda e, r=r, hs=hs, ps_=ps_: e.matmul(p_s, ktl[r][ps_, hs], itl[r][ps_, hs], start=True, stop=True), [("ktl", r), ("itl", r)], [("p_s", d)])
                    yield
                    fw.op("dve", lambda e, d=d, h=h, q=q, c=c: e.tensor_scalar(Sst[d][h][:], Sst[d][h][:], ex[q][:, 3 * c + 1:3 * c + 2], None, ALU.mult), [("Sst", d, h), ("ex", q)], [("Sst", d, h)])
                    yield
                    fw.op("dve", lambda e, d=d, h=h, q=q, c=c: e.scalar_tensor_tensor(Sst[d][h][:], p_s, ex[q][:, 3 * c + 2:3 * c + 3], Sst[d][h][:], ALU.mult, ALU.add), [("p_s", d), ("Sst", d, h), ("ex", q)], [("Sst", d, h)])
                    yield
            fw.op("act", lambda e, r=r: e.copy(hoo[r][:], p_o), [("p_o", d, 0), ("p_o", d, 1)], [("hoo", r)])
            yield
            fw.dma((d_ho if d == 0 else d_hob)[n * 128:(n + 1) * 128, :], hoo[r][:], [("hoo", r)], [("d_ho", d, n)])
            yield


            yield

        for it_ in range(NT):
            gens = [tile_body(0, it_), tile_body(1, it_)]
            live = [True, True]
            while any(live):
                for gi in range(2):
                    if live[gi]:
                        try:
                            next(gens[gi])
                        except StopIteration:
                            live[gi] = False
            if it_ % 16 == 15:
                fw.emit()
        NF = 4
        Rf = lambda name, sh, dt: [T("fin_%s%d" % (name, i), sh, dt) for i in range(NF)]
        hfw = Rf("hfw", [128, 256], F32); hbw = Rf("hbw", [128, 256], F32); osig = Rf("osig", [128, 256], F32)
        hofw = Rf("hofw", [128, 256], F32); hobw = Rf("hobw", [128, 256], F32); gsl = Rf("gsl", [128, 256], F32)
        hmo = Rf("hmo", [128, 256], F32); hsq = Rf("hsq", [128, 256], F32); ssq = Rf("ssq", [128, 1], F32); hgo = Rf("hgo", [128, 256], BF16)
        hoo = Rf("hoo", [128, 256], F32); hosq = Rf("hosq", [128, 256], F32); hssq = Rf("hssq", [128, 2], F32); hogo = Rf("hogo", [128, 256], BF16)

        def fin_body(n):
            r = n % NF
            rows = slice(n * 128, (n + 1) * 128)
            fw.dma(hfw[r][:], d_hm[rows, :], [("d_hm", 0, n)], [("hfw", r)])
            yield
            fw.dma(hbw[r][:], d_hmb[rows, :], [("d_hm", 1, n)], [("hbw", r)])
            yield
            fw.dma(osig[r][:], d_tm[rows, 256:512], [("d_tm", n)], [("osig", r)], eng="act")
            yield
            fw.dma(hofw[r][:], d_ho[rows, :], [("d_ho", 0, n)], [("hofw", r)])
            yield
            fw.dma(hobw[r][:], d_hob[rows, :], [("d_ho", 1, n)], [("hobw", r)])
            yield
            fw.dma(gsl[r][:], d_tm[rows, 768:1024], [("d_tm", n)], [("gsl", r)], eng="act")
            yield
            fw.op("dve", lambda e, r=r: e.tensor_tensor(hmo[r][:], hbw[r][:], hfw[r][:], ALU.add), [("hbw", r), ("hfw", r)], [("hmo", r)])
            yield
            fw.op("pool", lambda e, r=r: e.tensor_tensor(hsq[r][:], hmo[r][:], hmo[r][:], ALU.mult), [("hmo", r)], [("hsq", r)])
            yield
            fw.op("dve", lambda e, r=r: e.reduce_sum(ssq[r][:], hsq[r][:], AX.X), [("hsq", r)], [("ssq", r)])
            yield
            fw.op("dve", lambda e, r=r: e.tensor_scalar(ssq[r][:], ssq[r][:], 1.0 / 256, EPS, ALU.mult, ALU.add), [("ssq", r)], [("ssq", r)])
            yield
            fw.op("act", lambda e, r=r: e.activation(ssq[r][:], ssq[r][:], AF.Sqrt), [("ssq", r)], [("ssq", r)])
            yield
            fw.op("dve", lambda e, r=r: e.reciprocal(ssq[r][:], ssq[r][:]), [("ssq", r)], [("ssq", r)])
            yield
            fw.op("act", lambda e, r=r: e.activation(osig[r][:], osig[r][:], AF.Sigmoid), [("osig", r)], [("osig", r)])
            yield
            fw.op("dve", lambda e, r=r: e.scalar_tensor_tensor(hmo[r][:], hmo[r][:], ssq[r][:], gnm[:], ALU.mult, ALU.mult), [("hmo", r), ("ssq", r), "gnm"], [("hmo", r)])
            yield
            fw.op("dve", lambda e, r=r: e.tensor_tensor(hgo[r][:], hmo[r][:], osig[r][:], ALU.mult), [("hmo", r), ("osig", r)], [("hgo", r)])
            yield
            fw.dma(hg_out[rows, 0:256], hgo[r][:], [("hgo", r)], [("hg_out_m", n)])
            yield
            fw.op("dve", lambda e, r=r: e.tensor_tensor(hoo[r][:], hobw[r][:], hofw[r][:], ALU.add), [("hobw", r), ("hofw", r)], [("hoo", r)])
            yield
            fw.op("pool", lambda e, r=r: e.tensor_tensor(hosq[r][:], hoo[r][:], hoo[r][:], ALU.mult), [("hoo", r)], [("hosq", r)])
            yield
            fw.op("dve", lambda e, r=r: e.reduce_sum(hssq[r][:], hosq[r][:].rearrange("p (h v) -> p h v", h=2), AX.X), [("hosq", r)], [("hssq", r)])
            yield
            fw.op("dve", lambda e, r=r: e.tensor_scalar(hssq[r][:], hssq[r][:], 1.0 / 128, EPS, ALU.mult, ALU.add), [("hssq", r)], [("hssq", r)])
            yield
            fw.op("act", lambda e, r=r: e.activation(hssq[r][:], hssq[r][:], AF.Sqrt), [("hssq", r)], [("hssq", r)])
            yield
            fw.op("dve", lambda e, r=r: e.reciprocal(hssq[r][:], hssq[r][:]), [("hssq", r)], [("hssq", r)])
            yield
            fw.op("act", lambda e, r=r: e.activation(gsl[r][:], gsl[r][:], AF.Silu), [("gsl", r)], [("gsl", r)])
            yield
            for h in range(2):
                hs = slice(h * 128, (h + 1) * 128)
                fw.op("dve", lambda e, r=r, h=h, hs=hs: e.scalar_tensor_tensor(hoo[r][:, hs], hoo[r][:, hs], hssq[r][:, h:h + 1], gnh[:, hs], ALU.mult, ALU.mult), [("hoo", r), ("hssq", r), "gnh"], [("hoo", r)])
                yield
            fw.op("dve", lambda e, r=r: e.tensor_tensor(hogo[r][:], hoo[r][:], gsl[r][:], ALU.mult), [("hoo", r), ("gsl", r)], [("hogo", r)])
            yield
            fw.dma(hg_out[rows, 256:512], hogo[r][:], [("hogo", r)], [("hg_out_h", n)])
            yield

            yield

        for n0 in range(0, NT, NF):
            gens = [fin_body(n0 + j) for j in range(NF)]
            live = [True] * NF
            while any(live):
                for gi in range(NF):
                    if live[gi]:
                        try:
                            next(gens[gi])
                        except StopIteration:
                            live[gi] = False
        fw.emit()


TQ = 2048
NTQ = TQ // 128


def transpose16(fw, src_bf, src_key, identb, pT, dst, dst_keys, n_chunks=16, pkey=0):
    for half in range(n_chunks // 8):
        pt = pT[half % 2]
        for j in range(8):
            k = half * 8 + j
            fw.op("pe", lambda e, pt=pt, j=j, k=k: e.transpose(pt[:, j, :], src_bf[:, k * 128:(k + 1) * 128], identb[:]), [src_key, "identb"], [("pT", pkey, half % 2)])
        if half % 2 == 0:
            fw.op("act", lambda e, pt=pt, half=half: e.copy(dst[:, half * 8:(half + 1) * 8, :], pt[:]), [("pT", pkey, half % 2)], [dst_keys[half]])
        else:
            fw.op("dve", lambda e, pt=pt, half=half: e.tensor_copy(dst[:, half * 8:(half + 1) * 8, :], pt[:]), [("pT", pkey, half % 2)], [dst_keys[half]])


def build_phase_b(nc, fw, io):
    C = io["consts"]
    ag1 = io["ag1"]
    d_h0 = nc.dram_tensor("d_h0", [TQ, D], F32)
    d_hT = nc.dram_tensor("d_hT", [NTQ, 128, 16 * 128], BF16)
    d_gm = nc.dram_tensor("d_gm", [TQ, D], BF16)
    d_gh = nc.dram_tensor("d_gh", [TQ, D], BF16)
    d_mT = nc.dram_tensor("d_mT", [NTQ, 128, 16 * 128], BF16)
    d_h1 = io["d_h1"]
    h1b_out = io["h1b_out"]
    aff_out = io["aff_out"]

    for sub in range(2):
        with ExitStack() as st:
            T = lambda n, sh, dt: st.enter_context(nc.sbuf_tensor("b%d_%s" % (sub, n), sh, dt))
            P = lambda n, sh, dt: st.enter_context(nc.psum_tensor("pb%d_%s" % (sub, n), sh, dt))
            wg = T("wg", [128, 16, D], BF16)
            bg = T("bg", [128, D], F32)
            for k in range(16):
                fw.dma(wg[:, k, :], io["w_g"][k * 128:(k + 1) * 128, sub * D:(sub + 1) * D], [], [("wg", k)], eng="pool")
            fw.dma(bg[:], io["b_g"][:, sub * D:(sub + 1) * D], [], ["bg"])
            hT = [T("hT%d" % i, [128, 16, 128], BF16) for i in range(2)]
            gt = [T("gt%d" % i, [128, D], BF16) for i in range(2)]
            gtmp = [T("gtmp%d" % i, [128, 512], F32) for i in range(2)]
            pg = [P("pg%d" % i, [128, 512], F32) for i in range(2)]
            if sub == 0:
                lng = T("lng", [128, D], F32)
                lnb = T("lnb", [128, D], F32)
                identb = T("identb", [128, 128], BF16)
                fw.dma(lng[:], io["ln_g"], [], ["lng"])
                fw.dma(lnb[:], io["ln_b"], [], ["lnb"])
                fw.dma(identb[:], C["ident"], [], ["identb"], eng="pool")
                xt = [T("xt%d" % i, [128, D], F32) for i in range(2)]
                sq = T("sq", [128, D], F32)
                h0 = [T("h0%d" % i, [128, D], F32) for i in range(2)]
                hb = [T("hb%d" % i, [128, D], BF16) for i in range(2)]
                small = [T("sm%d" % i, [128, 1], F32) for i in range(6)]
                pT = [P("pT%d" % i, [128, 8, 128], BF16) for i in range(4)]
            def b12_body(t):
                r = t % 2
                rows = slice(t * 128, (t + 1) * 128)
                if sub == 0:
                    fw.dma(xt[r][:], io["xq"][rows, :], [], [("xt", r)])
                    yield
                    ln_rows(fw, T, xt[r][:], ("xt", r), lng[:], lnb[:], h0[r][:], ("h0", r), (sq,) + tuple(small))
                    yield
                    fw.dma(d_h0[rows, :], h0[r][:], [("h0", r)], [("d_h0", t)])
                    yield
                    fw.op("pool", lambda e, r=r: e.tensor_copy(hb[r][:], h0[r][:]), [("h0", r)], [("hb", r)])
                    yield
                    transpose16(fw, hb[r], ("hb", r), identb, pT[2 * r:2 * r + 2], hT[r], [("hT", r, 0), ("hT", r, 1)], pkey=r)
                    yield
                    fw.dma(d_hT[t], hT[r][:].rearrange("p a b -> p (a b)"), [("hT", r, 0), ("hT", r, 1)], [("d_hT", t)])
                    yield
                else:
                    fw.dma(hT[r][:].rearrange("p a b -> p (a b)"), d_hT[t], [("d_hT", t)], [("hT", r, 0), ("hT", r, 1)])
                    yield
                for cb in range(4):
                    pr = r
                    cs = slice(cb * 512, (cb + 1) * 512)
                    for k in range(16):
                        fw.op("pe", lambda e, pr=pr, k=k, r=r, cs=cs: e.matmul(pg[pr][:], hT[r][:, k, :], wg[:, k, cs], start=(k == 0), stop=(k == 15)),
                              [("hT", r, 0), ("hT", r, 1), ("wg", k)], [("pg", pr)])
                        yield
                    fw.op("dve", lambda e, pr=pr, cs=cs: e.tensor_tensor(gtmp[pr][:], pg[pr][:], bg[:, cs], ALU.add), [("pg", pr), "bg"], [("gtmp", pr)])
                    yield
                    fw.op("act", lambda e, pr=pr, r=r, cs=cs: e.activation(gt[r][:, cs], gtmp[pr][:], AF.Sigmoid), [("gtmp", pr)], [("gt", r, cb)])
                    yield
                dst = d_gm if sub == 0 else d_gh
                fw.dma(dst[rows, :], gt[r][:], [("gt", r, cb) for cb in range(4)], [("d_g", sub, t)])
                yield
                yield
            _items = list(range(NTQ))
            for _g0 in range(0, len(_items), 2):
                _gens = [b12_body(_v) for _v in _items[_g0:_g0 + 2]]
                _live = [True] * len(_gens)
                while any(_live):
                    for _gi in range(len(_gens)):
                        if _live[_gi]:
                            try:
                                next(_gens[_gi])
                            except StopIteration:
                                _live[_gi] = False
            fw.emit()

    with ExitStack() as st:
        T = lambda n, sh, dt: st.enter_context(nc.sbuf_tensor("b3_" + n, sh, dt))
        P = lambda n, sh, dt: st.enter_context(nc.psum_tensor("pb3_" + n, sh, dt))
        wbm = T("wbm", [128, 8, D], BF16)
        wbh = T("wbh", [128, 8, D], BF16)
        for k in range(8):
            fw.dma(wbm[:, k, :], io["w_bm"][k * 128:(k + 1) * 128, :], [], [("wbm", k)], eng="pool")
            fw.dma(wbh[:, k, :], io["w_bh"][k * 128:(k + 1) * 128, :], [], [("wbh", k)], eng="pool")
        identb = T("identb", [128, 128], BF16)
        fw.dma(identb[:], C["ident"], [], ["identb"], eng="pool")
        gidx = T("gidx", [128, NTQ, 4], I32)
        fw.dma(gidx[:], io["gidx"], [], ["gidx"])
        hgt = [T("hgt%d" % i, [128, 4, 512], BF16) for i in range(2)]
        hgT = [T("hgT%d" % i, [128, 16, 128], BF16) for i in range(2)]
        gm = [T("gm%d" % i, [128, D], BF16) for i in range(2)]
        gh = [T("gh%d" % i, [128, D], BF16) for i in range(2)]
        t1 = [T("t1%d" % i, [128, 512], F32) for i in range(2)]
        t2 = [T("t2%d" % i, [128, 512], F32) for i in range(2)]
        mg = [T("mg%d" % i, [128, D], BF16) for i in range(2)]
        mT = [T("mT%d" % i, [128, 16, 128], BF16) for i in range(2)]
        pT = [P("pT%d" % i, [128, 8, 128], BF16) for i in range(4)]
        pym = [P("pym%d" % i, [128, 512], F32) for i in range(2)]
        pyh = [P("pyh%d" % i, [128, 512], F32) for i in range(2)]
        def b3_body(t):
            r = t % 2
            rows = slice(t * 128, (t + 1) * 128)
            for hg in range(4):
                fw.op("pool", lambda e, r=r, hg=hg, t=t: e.indirect_dma_start(out=hgt[r][:, hg, :], out_offset=None, in_=ag1,
                                                                               in_offset=bass.IndirectOffsetOnAxis(ap=gidx[:, t, hg:hg + 1], axis=0)),
                      ["gidx", "ag1"], [("hgt", r, hg)], kind="d")
                yield
            fw.dma(gm[r][:], d_gm[rows, :], [("d_g", 0, t)], [("gm", r)])
            yield
            fw.dma(gh[r][:], d_gh[rows, :], [("d_g", 1, t)], [("gh", r)])
            yield
            for half in range(2):
                pt = pT[2 * r + half]
                for j in range(8):
                    kk = half * 8 + j
                    hg = (kk % 8) // 2
                    off = (0 if kk < 8 else 256) + (kk % 2) * 128
                    fw.op("pe", lambda e, pt=pt, j=j, hg=hg, off=off, r=r: e.transpose(pt[:, j, :], hgt[r][:, hg, off:off + 128], identb[:]),
                          [("hgt", r, hg), "identb"], [("pT", r, half)])
                    yield
                if half == 0:
                    fw.op("act", lambda e, pt=pt, r=r: e.copy(hgT[r][:, 0:8, :], pt[:]), [("pT", r, 0)], [("hgT", r, 0)])
                    yield
                else:
                    fw.op("dve", lambda e, pt=pt, r=r: e.tensor_copy(hgT[r][:, 8:16, :], pt[:]), [("pT", r, 1)], [("hgT", r, 1)])
                    yield
            for cb in range(4):
                pr = r
                cs = slice(cb * 512, (cb + 1) * 512)
                for k in range(8):
                    fw.op("pe", lambda e, pr=pr, k=k, r=r, cs=cs: e.matmul(pym[pr][:], hgT[r][:, k, :], wbm[:, k, cs], start=(k == 0), stop=(k == 7)),
                          [("hgT", r, 0), ("wbm", k)], [("pym", pr)])
                    yield
                for k in range(8):
                    fw.op("pe", lambda e, pr=pr, k=k, r=r, cs=cs: e.matmul(pyh[pr][:], hgT[r][:, 8 + k, :], wbh[:, k, cs], start=(k == 0), stop=(k == 7)),
                          [("hgT", r, 1), ("wbh", k)], [("pyh", pr)])
                    yield
                fw.op("dve", lambda e, pr=pr, r=r, cs=cs: e.tensor_tensor(t1[pr][:], pym[pr][:], gm[r][:, cs], ALU.mult), [("pym", pr), ("gm", r)], [("t1", pr)])
                yield
                fw.op("dve", lambda e, pr=pr, r=r, cs=cs: e.tensor_tensor(t2[pr][:], pyh[pr][:], gh[r][:, cs], ALU.mult), [("pyh", pr), ("gh", r)], [("t2", pr)])
                yield
                fw.op("pool", lambda e, pr=pr, r=r, cs=cs: e.tensor_tensor(mg[r][:, cs], t1[pr][:], t2[pr][:], ALU.add), [("t1", pr), ("t2", pr)], [("mg", r)])
                yield
            transpose16(fw, mg[r], ("mg", r), identb, pT[2 * r:2 * r + 2], mT[r], [("mT", r, 0), ("mT", r, 1)], pkey=r)
            yield
            fw.dma(d_mT[t], mT[r][:].rearrange("p a b -> p (a b)"), [("mT", r, 0), ("mT", r, 1)], [("d_mT", t)])
            yield
            yield
        _items = list(range(NTQ))
        for _g0 in range(0, len(_items), 2):
            _gens = [b3_body(_v) for _v in _items[_g0:_g0 + 2]]
            _live = [True] * len(_gens)
            while any(_live):
                for _gi in range(len(_gens)):
                    if _live[_gi]:
                        try:
                            next(_gens[_gi])
                        except StopIteration:
                            _live[_gi] = False
        fw.emit()

    with ExitStack() as st:
        T = lambda n, sh, dt: st.enter_context(nc.sbuf_tensor("b4_" + n, sh, dt))
        P = lambda n, sh, dt: st.enter_context(nc.psum_tensor("pb4_" + n, sh, dt))
        wo = T("wo", [128, 16, D], BF16)
        for k in range(16):
            fw.dma(wo[:, k, :], io["w_out"][k * 128:(k + 1) * 128, :], [], [("wo", k)], eng="pool")
        wr = T("wr", [128, 16, 16], F32)
        fw.dma(wr[:], io["w_router"].rearrange("(k p) e -> p k e", p=128), [], ["wr"])
        identf = T("identf", [128, 128], F32)
        fw.dma(identf[:], C["ident"], [], ["identf"])
        lng = T("lng", [128, D], F32)
        lnb = T("lnb", [128, D], F32)
        fw.dma(lng[:], io["ln1_g"], [], ["lng"])
        fw.dma(lnb[:], io["ln1_b"], [], ["lnb"])
        mT = [T("mT%d" % i, [128, 16, 128], BF16) for i in range(2)]
        h0 = [T("h0%d" % i, [128, D], F32) for i in range(2)]
        pre = [T("pre%d" % i, [128, D], F32) for i in range(2)]
        sq = T("sq", [128, D], F32)
        small = [T("sm%d" % i, [128, 1], F32) for i in range(6)]
        h1 = [T("h1%d" % i, [128, D], F32) for i in range(2)]
        h1b = [T("h1b%d" % i, [128, D], BF16) for i in range(2)]
        h1T = [T("h1T%d" % i, [128, 16, 128], F32) for i in range(2)]
        lg = [T("lg%d" % i, [128, 16], F32) for i in range(2)]
        mx = [T("mx%d" % i, [128, 1], F32) for i in range(2)]
        sm_ = [T("sms%d" % i, [128, 1], F32) for i in range(2)]
        pm = [P("pm%d" % i, [128, 512], F32) for i in range(2)]
        pTf = [P("pTf%d" % i, [128, 4, 128], F32) for i in range(4)]
        pl = [P("pl%d" % i, [128, 16], F32) for i in range(2)]
        def b4_body(t):
            r = t % 2
            rows = slice(t * 128, (t + 1) * 128)
            fw.dma(mT[r][:].rearrange("p a b -> p (a b)"), d_mT[t], [("d_mT", t)], [("mT", r)])
            yield
            fw.dma(h0[r][:], d_h0[rows, :], [("d_h0", t)], [("h0", r)])
            yield
            for cb in range(4):
                pr = r
                cs = slice(cb * 512, (cb + 1) * 512)
                for k in range(16):
                    fw.op("pe", lambda e, pr=pr, k=k, r=r, cs=cs: e.matmul(pm[pr][:], mT[r][:, k, :], wo[:, k, cs], start=(k == 0), stop=(k == 15)),
                          [("mT", r), ("wo", k)], [("pm", pr)])
                    yield
                fw.op("dve", lambda e, pr=pr, r=r, cs=cs: e.scalar_tensor_tensor(pre[r][:, cs], h0[r][:, cs], ALPHA, pm[pr][:], ALU.mult, ALU.add), [("h0", r), ("pm", pr)], [("pre", r)])
                yield
            ln_rows(fw, T, pre[r][:], ("pre", r), lng[:], lnb[:], h1[r][:], ("h1", r), (sq,) + tuple(small))
            yield
            fw.dma(d_h1[rows, :], h1[r][:], [("h1", r)], [("d_h1", t)])
            yield
            fw.op("pool", lambda e, r=r: e.tensor_copy(h1b[r][:], h1[r][:]), [("h1", r)], [("h1b", r)])
            yield
            fw.dma(h1b_out[rows, :], h1b[r][:], [("h1b", r)], [("h1b_out", t)])
            yield
            for g4 in range(4):
                pt = pTf[2 * r + g4 % 2]
                for j in range(4):
                    k = g4 * 4 + j
                    fw.op("pe", lambda e, pt=pt, j=j, k=k, r=r: e.transpose(pt[:, j, :], h1[r][:, k * 128:(k + 1) * 128], identf[:]), [("h1", r), "identf"], [("pTf", r, g4 % 2)])
                    yield
                fw.op("act" if g4 % 2 == 0 else "dve", (lambda e, pt=pt, g4=g4: e.copy(h1T[r][:, g4 * 4:(g4 + 1) * 4, :], pt[:])) if g4 % 2 == 0 else
                      (lambda e, pt=pt, g4=g4: e.tensor_copy(h1T[r][:, g4 * 4:(g4 + 1) * 4, :], pt[:])), [("pTf", r, g4 % 2)], [("h1T", r, g4)])
                yield
            for k in range(16):
                fw.op("pe", lambda e, k=k: e.matmul(pl[r][:], h1T[r][:, k, :], wr[:, k, :], start=(k == 0), stop=(k == 15)), [("h1T", r, k // 4), "wr"], [("pl", r)])
                yield
            fw.op("dve", lambda e, r=r: e.reduce_max(mx[r][:], pl[r][:], AX.X), [("pl", r)], [("mx", r)])
            yield
            fw.op("dve", lambda e, r=r: e.tensor_scalar(mx[r][:], mx[r][:], -1.0, None, ALU.mult), [("mx", r)], [("mx", r)])
            yield
            fw.op("act", lambda e, r=r: e.activation(lg[r][:], pl[r][:], AF.Exp, bias=mx[r][:]), [("pl", r), ("mx", r)], [("lg", r)])
            yield
            fw.op("dve", lambda e, r=r: e.reduce_sum(sm_[r][:], lg[r][:], AX.X), [("lg", r)], [("sms", r)])
            yield
            fw.op("dve", lambda e, r=r: e.reciprocal(sm_[r][:], sm_[r][:]), [("sms", r)], [("sms", r)])
            yield
            fw.op("dve", lambda e, r=r: e.tensor_scalar(lg[r][:], lg[r][:], sm_[r][:], None, ALU.mult), [("lg", r), ("sms", r)], [("lg", r)])
            yield
            fw.dma(aff_out[rows, :], lg[r][:], [("lg", r)], [("aff_out", t)])
            yield
            yield
        _items = list(range(NTQ))
        for _g0 in range(0, len(_items), 2):
            _gens = [b4_body(_v) for _v in _items[_g0:_g0 + 2]]
            _live = [True] * len(_gens)
            while any(_live):
                for _gi in range(len(_gens)):
                    if _live[_gi]:
                        try:
                            next(_gens[_gi])
                        except StopIteration:
                            _live[_gi] = False
        fw.emit()


DFF = 5632
NFB = DFF // 128
CAP = 1024
TQ = 2048
NTQ = 16
NBIS = 34

C_CONST_SHAPES = {"sl": [128, 128], "iota": [128, 1024], "tokinfo": [128, 64, 2]}


def host_consts_c():
    s = np.arange(128)[:, None]
    t = np.arange(128)[None, :]
    c = {}
    c["sl"] = (s < t).astype(np.float32)
    c["iota"] = np.ascontiguousarray(np.broadcast_to(np.arange(1024, dtype=np.float32)[None, :], (128, 1024)))
    ti = np.zeros((128, 64, 2), np.float32)
    ti[:, :, 0] = np.arange(128)[:, None]
    ti[:, :, 1] = np.arange(64)[None, :]
    c["tokinfo"] = ti
    return c


def build_phase_c(nc, fw, io):
    C = io["consts"]
    ag2 = io["ag2"]
    ag_aff = io["ag_aff"]
    ye_out = io["ye_out"]
    idx_out = io["idx_out"]
    d_idx = nc.dram_tensor("d_idx", [128, 4, 8], I32)
    d_gsel = nc.dram_tensor("d_gsel", [128, 4, 8], F32)

    with ExitStack() as st:
        T = lambda n, sh, dt: st.enter_context(nc.sbuf_tensor("c0_" + n, sh, dt))
        P = lambda n, sh, dt: st.enter_context(nc.psum_tensor("pc0_" + n, sh, dt))
        ones = T("ones", [128, 128], F32)
        sl = T("sl", [128, 128], F32)
        iota = T("iota", [128, 1024], F32)
        tokinfo = T("tokinfo", [128, 64, 2], BF16)
        selm = T("selm", [128, 2, 1, 16], F32)
        fw.dma(ones[:], C["ones"], [], ["ones"])
        fw.dma(sl[:], C["sl"], [], ["sl"])
        fw.dma(iota[:], C["iota"], [], ["iota"])
        fw.dma(tokinfo[:], C["tokinfo"], [], ["tokinfo"], eng="pool")
        fw.dma(selm[:], io["selm"], [], ["selm"])
        affb = [T("affb%d" % i, [128, 64, 16], F32) for i in range(2)]
        tmp = T("tmp", [128, 64, 16], F32)
        vals = T("vals", [128, 4, 64], F32)
        lo = T("lo", [128, 4], F32)
        hi = T("hi", [128, 4], F32)
        mid = T("mid", [128, 4], F32)
        cmp = [T("cmp%d" % i, [128, 64], F32) for i in range(2)]
        cntp = T("cntp", [128, 4], F32)
        ge = T("ge", [128, 4], F32)
        dd = T("dd", [128, 4], F32)
        d2 = T("d2", [128, 4], F32)
        m = T("m", [128, 4, 64], F32)
        onesrow = T("onesrow", [128, 64], F32)
        incl = T("incl", [128, 4, 64], F32)
        pos = T("pos", [128, 4, 64], F32)
        rowtot = T("rowtot", [128, 4], F32)
        offs = T("offs", [128, 4], F32)
        oneh = [T("oneh%d" % i, [128, 1024], BF16) for i in range(2)]
        idxf = T("idxf", [128, 4, 8], F32)
        pidxs = T("pidxs", [128, 8, 2], F32)
        idxi = T("idxi", [128, 4, 8], I32)
        ptot = P("ptot", [128, 4], F32)
        poffs = P("poffs", [128, 4], F32)
        pidx = P("pidx", [128, 8, 2], F32)
        for b in range(2):
            fw.dma(affb[b][:], ag_aff[b * S:(b + 1) * S, :].rearrange("(p f) e -> p f e", p=128), ["ag_aff"], [("affb", b)])
            for el in range(2):
                L = b * 2 + el
                fw.op("dve", lambda e, b=b, el=el: e.tensor_tensor(tmp[:], affb[b][:], selm[:, el, :, :].to_broadcast([128, 64, 16]), ALU.mult), [("affb", b), "selm"], ["tmp"])
                fw.op("dve", lambda e, L=L: e.reduce_sum(vals[:, L, :], tmp[:], AX.X), ["tmp"], ["vals"])
        fw.op("dve", lambda e: e.memset(lo[:], 0.0), [], ["lo"])
        fw.op("dve", lambda e: e.memset(hi[:], 1.0), [], ["hi"])
        fw.op("dve", lambda e: e.memset(onesrow[:], 1.0), [], ["onesrow"])
        for it in range(NBIS):
            fw.op("dve", lambda e: e.tensor_tensor(mid[:], lo[:], hi[:], ALU.add), ["lo", "hi"], ["mid"])
            fw.op("dve", lambda e: e.tensor_scalar(mid[:], mid[:], 0.5, None, ALU.mult), ["mid"], ["mid"])
            for L in range(4):
                fw.op("dve", lambda e, L=L: e.tensor_scalar(cmp[L % 2][:], vals[:, L, :], mid[:, L:L + 1], None, ALU.is_gt), ["vals", "mid"], [("cmp", L % 2)])
                fw.op("dve", lambda e, L=L: e.reduce_sum(cntp[:, L:L + 1], cmp[L % 2][:], AX.X), [("cmp", L % 2)], ["cntp"])
            fw.op("pe", lambda e: e.matmul(ptot[:], ones[:], cntp[:], start=True, stop=True), ["ones", "cntp"], ["ptot"])
            fw.op("dve", lambda e: e.tensor_scalar(ge[:], ptot[:], CAP - 0.5, None, ALU.is_ge), ["ptot"], ["ge"])
            fw.op("dve", lambda e: e.tensor_tensor(dd[:], mid[:], lo[:], ALU.subtract), ["mid", "lo"], ["dd"])
            fw.op("dve", lambda e: e.tensor_tensor(dd[:], dd[:], ge[:], ALU.mult), ["dd", "ge"], ["dd"])
            fw.op("dve", lambda e: e.tensor_tensor(d2[:], hi[:], mid[:], ALU.subtract), ["mid", "hi"], ["d2"])
            fw.op("dve", lambda e: e.tensor_tensor(d2[:], d2[:], ge[:], ALU.mult), ["d2", "ge"], ["d2"])
            fw.op("dve", lambda e: e.tensor_tensor(lo[:], lo[:], dd[:], ALU.add), ["lo", "dd"], ["lo"])
            fw.op("dve", lambda e: e.tensor_tensor(hi[:], mid[:], d2[:], ALU.add), ["mid", "d2"], ["hi"])
        for L in range(4):
            fw.op("dve", lambda e, L=L: e.tensor_scalar(m[:, L, :], vals[:, L, :], lo[:, L:L + 1], None, ALU.is_gt), ["vals", "lo"], ["m"])
            fw.op("dve", lambda e, L=L: e.tensor_tensor_scan(incl[:, L, :], onesrow[:], m[:, L, :], 0.0, ALU.mult, ALU.add), ["m", "onesrow"], ["incl"])
            fw.op("dve", lambda e, L=L: e.tensor_copy(rowtot[:, L:L + 1], incl[:, L, 63:64]), ["incl"], ["rowtot"])
        fw.op("pe", lambda e: e.matmul(poffs[:], sl[:], rowtot[:], start=True, stop=True), ["sl", "rowtot"], ["poffs"])
        fw.op("dve", lambda e: e.tensor_copy(offs[:], poffs[:]), ["poffs"], ["offs"])
        fw.op("dve", lambda e: e.tensor_tensor(pos[:], incl[:], m[:], ALU.subtract), ["incl", "m"], ["pos"])
        for L in range(4):
            fw.op("dve", lambda e, L=L: e.tensor_scalar(pos[:, L, :], pos[:, L, :], offs[:, L:L + 1], None, ALU.add), ["pos", "offs"], ["pos"])
        zb = T("zb", [128, 128], BF16)
        fw.op("pool", lambda e: e.memset(zb[:], 0.0), [], ["zb"])
        for L in range(4):
            b = L // 2
            fw.op("pe", lambda e: e.matmul(pidx[:].rearrange("p a b -> p (a b)"), zb[:], tokinfo[:, 0:8, :].rearrange("p a b -> p (a b)"), start=True, stop=False), ["zb", "tokinfo", "pidxs"], ["pidx"])
            for f in range(64):
                r = f % 2
                fw.op("dve", lambda e, L=L, f=f, r=r: e.tensor_scalar(oneh[r][:], iota[:], pos[:, L, f:f + 1], m[:, L, f:f + 1], ALU.is_equal, ALU.mult), ["iota", "pos", "m"], [("oneh", r)])
                for jb in range(8):
                    fw.op("pe", lambda e, jb=jb, f=f, r=r: e.matmul(pidx[:, jb, :], oneh[r][:, jb * 128:(jb + 1) * 128], tokinfo[:, f, :], start=False, stop=(f == 63)),
                          [("oneh", r), "tokinfo"], ["pidx"])
            fw.op("dve", lambda e: e.tensor_copy(pidxs[:], pidx[:]), ["pidx"], ["pidxs"])
            fw.op("dve", lambda e, L=L: e.scalar_tensor_tensor(idxf[:, L, :], pidxs[:, :, 0], 64.0, pidxs[:, :, 1], ALU.mult, ALU.add), ["pidxs"], [("idxf", L)])
            fw.op("dve", lambda e, L=L, b=b: e.tensor_scalar(idxf[:, L, :], idxf[:, L, :], float(b * S), None, ALU.add), [("idxf", L)], [("idxf", L)])
        fw.op("dve", lambda e: e.tensor_copy(idxi[:], idxf[:]), [("idxf", L) for L in range(4)], ["idxi"])
        fw.dma(d_idx[:, :, :], idxi[:], ["idxi"], ["d_idx"])
        for L in range(4):
            fw.dma(idx_out[L * CAP:(L + 1) * CAP, :].rearrange("(j p) o -> p (j o)", p=128), idxi[:, L, :], ["idxi"], [("idx_out", L)], allow_slow_non_contiguous=True)
        fw.emit()

    with ExitStack() as st0:
        T0 = lambda n, sh, dt: st0.enter_context(nc.sbuf_tensor("c1_" + n, sh, dt))
        hidT = T0("hidT", [128, NFB, CAP], BF16)
        idxi = T0("idxi", [128, 4, 8], I32)
        selm = T0("selm", [128, 2, 1, 16], F32)
        identb = T0("identb", [128, 128], BF16)
        gsel = T0("gsel", [128, 4, 8], F32)
        fw.dma(idxi[:], d_idx[:, :, :], ["d_idx"], ["idxi1"])
        fw.dma(selm[:], io["selm"], [], ["selm1"])
        fw.dma(identb[:], C["ident"], [], ["identb"], eng="pool")
        for L in range(4):
            b, el = L // 2, L % 2
            with ExitStack() as st:
                T = lambda n, sh, dt: st.enter_context(nc.sbuf_tensor("c1a%d_%s" % (L, n), sh, dt))
                P = lambda n, sh, dt: st.enter_context(nc.psum_tensor("pc1a%d_%s" % (L, n), sh, dt))
                xe = [T("xe%d" % i, [128, D], BF16) for i in range(2)]
                xeT = T("xeT", [128, 16, CAP], BF16)
                gat = T("gat", [128, 8, 16], F32)
                g16 = T("g16", [128, 16], F32)
                wgt = [T("wgt%d" % i, [128, 16, 128], BF16) for i in range(2)]
                wut = [T("wut%d" % i, [128, 16, 128], BF16) for i in range(2)]
                sg = [T("sg%d" % i, [128, 512], F32) for i in range(2)]
                pT = [P("pT%d" % i, [128, 8, 128], BF16) for i in range(2)]
                pgt = [P("pgt%d" % i, [128, 512], F32) for i in range(2)]
                put = [P("put%d" % i, [128, 512], F32) for i in range(2)]
                for jb in range(8):
                    r = jb % 2
                    fw.op("pool", lambda e, r=r, jb=jb, L=L: e.indirect_dma_start(out=xe[r][:], out_offset=None, in_=ag2,
                                                                                   in_offset=bass.IndirectOffsetOnAxis(ap=idxi[:, L, jb:jb + 1], axis=0)),
                          ["idxi1", "ag2"], [("xe", r)], kind="d")
                    fw.op("pool", lambda e, jb=jb, L=L: e.indirect_dma_start(out=gat[:, jb, :], out_offset=None, in_=ag_aff,
                                                                              in_offset=bass.IndirectOffsetOnAxis(ap=idxi[:, L, jb:jb + 1], axis=0)),
                          ["idxi1", "ag_aff"], [("gat", jb)], kind="d")
                    for half in range(2):
                        pt = pT[half]
                        for j in range(8):
                            k = half * 8 + j
                            fw.op("pe", lambda e, pt=pt, j=j, k=k, r=r: e.transpose(pt[:, j, :], xe[r][:, k * 128:(k + 1) * 128], identb[:]), [("xe", r), "identb"], [("pT", half)])
                        if half == 0:
                            fw.op("act", lambda e, pt=pt, jb=jb: e.copy(xeT[:, 0:8, jb * 128:(jb + 1) * 128], pt[:]), [("pT", 0)], [("xeT", jb, 0)])
                        else:
                            fw.op("dve", lambda e, pt=pt, jb=jb: e.tensor_copy(xeT[:, 8:16, jb * 128:(jb + 1) * 128], pt[:]), [("pT", 1)], [("xeT", jb, 1)])
                    fw.op("dve", lambda e, jb=jb, el=el: e.tensor_tensor(g16[:], gat[:, jb, :], selm[:, el, 0, :], ALU.mult), [("gat", jb), "selm1"], ["g16"])
                    fw.op("dve", lambda e, jb=jb, L=L: e.reduce_sum(gsel[:, L, jb:jb + 1], g16[:], AX.X), ["g16"], [("gsel", L, jb)])
                xk = [("xeT", jb, h) for jb in range(8) for h in range(2)]
                for fb in range(NFB):
                    r = fb % 2
                    fs = slice(fb * 128, (fb + 1) * 128)
                    fw.dma(wgt[r][:], io["w_gate"][el, :, fs].rearrange("(k p) f -> p k f", p=128), [], [("wgt", r)], eng="pool")
                    fw.dma(wut[r][:], io["w_up"][el, :, fs].rearrange("(k p) f -> p k f", p=128), [], [("wut", r)], eng="pool")
                    for tb in range(2):
                        ts_ = slice(tb * 512, (tb + 1) * 512)
                        for k in range(16):
                            fw.op("pe", lambda e, tb=tb, k=k, r=r, ts_=ts_: e.matmul(pgt[tb][:], wgt[r][:, k, :], xeT[:, k, ts_], start=(k == 0), stop=(k == 15)),
                                  [("wgt", r)] + (xk if fb == 0 else []), [("pgt", tb)])
                        for k in range(16):
                            fw.op("pe", lambda e, tb=tb, k=k, r=r, ts_=ts_: e.matmul(put[tb][:], wut[r][:, k, :], xeT[:, k, ts_], start=(k == 0), stop=(k == 15)),
                                  [("wut", r)], [("put", tb)])
                        fw.op("act", lambda e, tb=tb: e.activation(sg[tb][:], pgt[tb][:], AF.Silu), [("pgt", tb)], [("sg", tb)])
                        fw.op("dve", lambda e, tb=tb, fb=fb, ts_=ts_: e.tensor_tensor(hidT[:, fb, ts_], sg[tb][:], put[tb][:], ALU.mult), [("sg", tb), ("put", tb)], [("hidT", fb)])
                fw.emit()
            with ExitStack() as st:
                T = lambda n, sh, dt: st.enter_context(nc.sbuf_tensor("c1b%d_%s" % (L, n), sh, dt))
                P = lambda n, sh, dt: st.enter_context(nc.psum_tensor("pc1b%d_%s" % (L, n), sh, dt))
                wd = [T("wd%d" % i, [128, NFB, 256], BF16) for i in range(2)]
                yo = [T("yo%d" % i, [128, 256], F32) for i in range(2)]
                py = [P("py%d" % i, [128, 256], F32) for i in range(2)]
                hk = [("hidT", fb) for fb in range(NFB)]
                cnt = 0
                for dmb in range(8):
                    r = dmb % 2
                    cs = slice(dmb * 256, (dmb + 1) * 256)
                    for hf in range(2):
                        fw.dma(wd[r][:, hf * 22:(hf + 1) * 22, :], io["w_down"][el, hf * 22 * 128:(hf + 1) * 22 * 128, cs].rearrange("(k p) c -> p k c", p=128), [], [("wd", r, hf)], eng="pool")
                    for jb in range(8):
                        q = cnt % 2
                        cnt += 1
                        for fc in range(NFB):
                            fw.op("pe", lambda e, q=q, fc=fc, jb=jb, r=r: e.matmul(py[q][:], hidT[:, fc, jb * 128:(jb + 1) * 128], wd[r][:, fc, :], start=(fc == 0), stop=(fc == NFB - 1)),
                                  [("wd", r, fc // 22)] + (hk if ((dmb == 0 and jb == 0 and fc == 0) or (dmb == 7 and jb == 7 and fc == NFB - 1)) else []), [("py", q)])
                        fw.op("act", lambda e, q=q, jb=jb, L=L: e.activation(yo[q][:], py[q][:], AF.Copy, scale=gsel[:, L, jb:jb + 1]), [("py", q), ("gsel", L, jb)], [("yo", q)])
                        fw.dma(ye_out[L * CAP + jb * 128:L * CAP + (jb + 1) * 128, cs], yo[q][:], [("yo", q)], [("ye_out", L, jb, dmb)])
                fw.emit()


def build_phase_d(nc, fw, io):
    C = io["consts"]
    ag3 = io["ag3"]
    ag_idx = io["ag_idx"]
    d_h1 = io["d_h1"]
    out = io["out"]
    ffn = nc.dram_tensor("ffn_acc", [TQ + 128, D], F32)
    NG = 128
    with ExitStack() as st:
        T = lambda n, sh, dt: st.enter_context(nc.sbuf_tensor("d_" + n, sh, dt))
        zero = T("zero", [128, D], F32)
        fw.op("pool", lambda e: e.memset(zero[:], 0.0), [], ["zero"])
        for t in range(NTQ):
            fw.dma(ffn[t * 128:(t + 1) * 128, :], zero[:], ["zero"], ["ffn"])
        drow = T("drow", [128, NG], I32)
        basef = T("basef", [128, 1], F32)
        fw.dma(drow[:], io["drow"], [], ["drow"])
        fw.dma(basef[:], io["basef"], [], ["basef"])
        trash = T("trash", [128, 1], F32)
        fw.dma(trash[:], io["trash"], [], ["trash"])
        toki = T("toki", [128, NG], I32)
        tokf = T("tokf", [128, NG], F32)
        drowf = T("drowf", [128, NG], F32)
        v1 = T("v1", [128, NG], F32)
        v2 = T("v2", [128, NG], F32)
        idl = T("idl", [128, NG], F32)
        idli = T("idli", [128, NG], I32)
        grf = T("grf", [128, NG], F32)
        gri = T("gri", [128, NG], I32)
        yg = [T("yg%d" % i, [128, D], F32) for i in range(3)]
        idc = [T("idc%d" % i, [128, 1], I32) for i in range(3)]
        for g in range(NG):
            fw.op("pool", lambda e, g=g: e.indirect_dma_start(out=toki[:, g:g + 1], out_offset=None, in_=ag_idx,
                                                               in_offset=bass.IndirectOffsetOnAxis(ap=drow[:, g:g + 1], axis=0)),
                  ["drow", "ag_idx"], [("toki", g)], kind="d")
        tk = [("toki", g) for g in range(NG)]
        BIG = 100000.0
        BIGR = 1000000.0
        fw.op("dve", lambda e: e.tensor_copy(tokf[:], toki[:]), tk, ["tokf"])
        fw.op("dve", lambda e: e.tensor_copy(drowf[:], drow[:]), ["drow"], ["drowf"])
        fw.op("dve", lambda e: e.tensor_scalar(tokf[:], tokf[:], basef[:, 0:1], None, ALU.subtract), ["tokf", "basef"], ["tokf"])
        fw.op("dve", lambda e: e.tensor_scalar(v1[:], tokf[:], -0.5, None, ALU.is_ge), ["tokf"], ["v1"])
        fw.op("dve", lambda e: e.tensor_scalar(v2[:], tokf[:], TQ - 0.5, None, ALU.is_lt), ["tokf"], ["v2"])
        fw.op("dve", lambda e: e.tensor_tensor(v1[:], v1[:], v2[:], ALU.mult), ["v1", "v2"], ["v1"])
        fw.op("dve", lambda e: e.tensor_scalar(idl[:], tokf[:], trash[:, 0:1], None, ALU.subtract), ["tokf", "trash"], ["idl"])
        fw.op("dve", lambda e: e.tensor_tensor(idl[:], idl[:], v1[:], ALU.mult), ["idl", "v1"], ["idl"])
        fw.op("dve", lambda e: e.tensor_scalar(idl[:], idl[:], trash[:, 0:1], None, ALU.add), ["idl", "trash"], ["idl"])
        fw.op("dve", lambda e: e.tensor_copy(idli[:], idl[:]), ["idl"], ["idli"])
        for g in range(NG):
            r = g % 3
            fw.op("pool", lambda e, g=g, r=r: e.indirect_dma_start(out=yg[r][:], out_offset=None, in_=ag3,
                                                                    in_offset=bass.IndirectOffsetOnAxis(ap=drow[:, g:g + 1], axis=0)),
                  ["drow", "ag3"], [("yg", r)], kind="d")
            fw.op("dve", lambda e, g=g, r=r: e.tensor_copy(idc[r][:], idli[:, g:g + 1]), ["idli"], [("idc", r)])
            fw.op("pool", lambda e, g=g, r=r: e.indirect_dma_start(out=ffn[:, :], out_offset=bass.IndirectOffsetOnAxis(ap=idc[r][:, :], axis=0),
                                                                    in_=yg[r][:], in_offset=None, compute_op=ALU.add),
                  [("idc", r), ("yg", r), "ffn"], ["ffn"], kind="d")
            if g % 16 == 15:
                fw.emit()
        fw.emit()
    with ExitStack() as st:
        T = lambda n, sh, dt: st.enter_context(nc.sbuf_tensor("d2_" + n, sh, dt))
        lng = T("lng", [128, D], F32)
        lnb = T("lnb", [128, D], F32)
        fw.dma(lng[:], io["ln2_g"], [], ["lng"])
        fw.dma(lnb[:], io["ln2_b"], [], ["lnb"])
        h1 = [T("h1%d" % i, [128, D], F32) for i in range(2)]
        ff = [T("ff%d" % i, [128, D], F32) for i in range(2)]
        pre = [T("pre%d" % i, [128, D], F32) for i in range(2)]
        oo = [T("oo%d" % i, [128, D], F32) for i in range(2)]
        sq = T("sq", [128, D], F32)
        small = [T("sm%d" % i, [128, 1], F32) for i in range(6)]
        for t in range(NTQ):
            r = t % 2
            rows = slice(t * 128, (t + 1) * 128)
            fw.dma(h1[r][:], d_h1[rows, :], [("d_h1", t)], [("h1", r)])
            fw.dma(ff[r][:], ffn[rows, :], ["ffn"], [("ff", r)])
            fw.op("dve", lambda e, r=r: e.scalar_tensor_tensor(pre[r][:], h1[r][:], ALPHA, ff[r][:], ALU.mult, ALU.add), [("h1", r), ("ff", r)], [("pre", r)])
            ln_rows(fw, T, pre[r][:], ("pre", r), lng[:], lnb[:], oo[r][:], ("oo", r), (sq,) + tuple(small))
            fw.dma(out[rows, :], oo[r][:], [("oo", r)], [("out", t)])
        fw.emit()

QK_M=512; V_M=1024; NH_M=4; Q_H=1024; V_H=1024
OFF={}
_o=0
for name,sz in [("qk",1024),("v",1024),("o",1024),("ig",8),("fg",8),("qh",1024),("fh",2048),("ih",1024),("gh",1024),("gm",2048),("gH",2048)]:
    OFF[name]=_o; _o+=sz

def cols_for(hg):
    r=np.arange
    fm=np.concatenate([OFF["qk"]+hg*128+r(128), OFF["qk"]+512+hg*128+r(128), OFF["qh"]+hg*256+r(256)])
    tm=np.concatenate([OFF["v"]+hg*256+r(256), OFF["o"]+hg*256+r(256), OFF["ih"]+hg*256+r(256), OFF["gh"]+hg*256+r(256),
                       OFF["fh"]+hg*256+r(256), OFF["fh"]+1024+hg*256+r(256),
                       [OFF["ig"]+hg, OFF["ig"]+4+hg, OFF["fg"]+hg, OFF["fg"]+4+hg]]).astype(np.int64)
    return fm,tm

def prep_A(inp, core, consts):
    b=core//4; hg=core%4
    fm,tm=cols_for(hg)
    w=inp["w_in"][0]; bi=inp["b_in"][0]
    d={}
    d["xb"]=np.ascontiguousarray(inp["x"][b])
    d["wfm"]=np.ascontiguousarray(w[:,fm]); d["wtm"]=np.ascontiguousarray(w[:,tm])
    d["bfm"]=np.ascontiguousarray(bi[fm].reshape(4,128).T.reshape(128,4,1))
    d["btm"]=np.ascontiguousarray(np.broadcast_to(bi[tm][None,:],(128,1540)))
    d["ln_g"]=np.ascontiguousarray(np.broadcast_to(inp["ln_in_g"][None,:],(128,2048)))
    d["ln_b"]=np.ascontiguousarray(np.broadcast_to(inp["ln_in_b"][None,:],(128,2048)))
    cw=inp["conv_w"][0]
    d["cw"]=np.ascontiguousarray(np.stack([cw[:,hg*128:(hg+1)*128].T, cw[:,512+hg*128:512+(hg+1)*128].T],1))
    d["gn_m"]=np.ascontiguousarray(np.broadcast_to(inp["mlstm_norm_g"][0,hg][None,:],(128,256)))
    d["gn_h"]=np.ascontiguousarray(np.broadcast_to(inp["hgrn_norm_g"][0,2*hg:2*hg+2].reshape(1,256),(128,256)))
    lg=inp["hgrn_lb_logits"]
    sel=lambda slot: np.concatenate([lg[dd,slot,hg*256:(hg+1)*256] for dd in range(2)])
    d["lb0"]=np.ascontiguousarray(np.broadcast_to(sel(0)[None,:],(128,512)))
    d["lb1"]=np.ascontiguousarray(np.broadcast_to(sel(1)[None,:],(128,512)))
    for k,v in consts.items(): d["c_"+k]=v
    return d

def prep_B(inp, core):
    b=core//4; q=core%4
    w=inp["w_in"][0]; bi=inp["b_in"][0]
    d={}
    d["xq"]=np.ascontiguousarray(inp["x"][b, q*2048:(q+1)*2048])
    d["w_g"]=np.ascontiguousarray(w[:, OFF["gm"]:OFF["gm"]+4096])
    d["b_g"]=np.ascontiguousarray(np.broadcast_to(bi[OFF["gm"]:OFF["gm"]+4096][None,:],(128,4096)))
    d["w_bm"]=np.ascontiguousarray(inp["w_branch_m"][0]); d["w_bh"]=np.ascontiguousarray(inp["w_branch_h"][0])
    d["w_out"]=np.ascontiguousarray(inp["w_out"][0])
    d["ln1_g"]=np.ascontiguousarray(np.broadcast_to(inp["ln1_g"][0][None,:],(128,2048)))
    d["ln1_b"]=np.ascontiguousarray(np.broadcast_to(inp["ln1_b"][0][None,:],(128,2048)))
    d["w_router"]=np.ascontiguousarray(inp["w_router"][0])
    p=np.arange(128)[:,None,None]; t=np.arange(16)[None,:,None]; hg=np.arange(4)[None,None,:]
    d["gidx"]=((4*b+hg)*8192 + q*2048 + t*128 + p).astype(np.int32)
    return d

def prep_CD(inp, core):
    b=core//4; q=core%4
    d={}
    sel=np.zeros((128,2,1,16),np.float32)
    for el in range(2): sel[:,el,0,2*core+el]=1.0
    d["selm"]=sel
    d["w_gate"]=np.ascontiguousarray(inp["w_gate_e"][0,2*core:2*core+2])
    d["w_up"]=np.ascontiguousarray(inp["w_up_e"][0,2*core:2*core+2])
    d["w_down"]=np.ascontiguousarray(inp["w_down_e"][0,2*core:2*core+2])
    p=np.arange(128)[:,None]
    g=np.arange(128)[None,:]
    r=g//16; el=(g%16)//8; jb=g%8
    d["drow"]=(r*4096+(b*2+el)*1024+jb*128+p).astype(np.int32)
    d["basef"]=np.full((128,1),b*8192+q*2048,np.float32)
    d["trash"]=(2048+np.arange(128,dtype=np.float32)).reshape(128,1)
    d["ln2_g"]=np.ascontiguousarray(np.broadcast_to(inp["ln2_g"][0][None,:],(128,2048)))
    d["ln2_b"]=np.ascontiguousarray(np.broadcast_to(inp["ln2_b"][0][None,:],(128,2048)))
    return d

def build_full():
    nc = bass.Bass("TRN2", target_bir_lowering=False)
    io={}
    def inp(name,shape,dt=F32):
        io[name]=nc.dram_tensor(name,shape,dt,kind="ExternalInput").ap()
    inp("xb",[8192,2048]); inp("wfm",[2048,512]); inp("wtm",[2048,1540]); inp("bfm",[128,4,1]); inp("btm",[128,1540])
    inp("ln_g",[128,2048]); inp("ln_b",[128,2048]); inp("cw",[128,2,5]); inp("gn_m",[128,256]); inp("gn_h",[128,256])
    inp("lb0",[128,512]); inp("lb1",[128,512])
    inp("xq",[2048,2048]); inp("w_g",[2048,4096]); inp("b_g",[128,4096]); inp("w_bm",[1024,2048]); inp("w_bh",[1024,2048]); inp("w_out",[2048,2048])
    inp("ln1_g",[128,2048]); inp("ln1_b",[128,2048]); inp("w_router",[2048,16]); inp("gidx",[128,16,4],I32)
    inp("selm",[128,2,1,16]); inp("w_gate",[2,2048,5632]); inp("w_up",[2,2048,5632]); inp("w_down",[2,5632,2048])
    inp("drow",[128,128],I32); inp("basef",[128,1]); inp("trash",[128,1]); inp("ln2_g",[128,2048]); inp("ln2_b",[128,2048])
    io["consts"]={}
    shapes=dict(CONST_SHAPES); shapes.update(C_CONST_SHAPES)
    for k,sh in shapes.items():
        io["consts"][k]=nc.dram_tensor("c_"+k,sh,F32,kind="ExternalInput").ap()
    hg=nc.dram_tensor("hg_out",[8192,512],BF16); io["hg_out"]=hg
    ag1=nc.dram_tensor("ag1",[65536,512],BF16); io["ag1"]=ag1.ap()
    io["d_h1"]=nc.dram_tensor("d_h1",[2048,2048],F32)
    h1b=nc.dram_tensor("h1b_out",[2048,2048],BF16); io["h1b_out"]=h1b
    affo=nc.dram_tensor("aff_out",[2048,16],F32); io["aff_out"]=affo
    ag2=nc.dram_tensor("ag2",[16384,2048],BF16); io["ag2"]=ag2.ap()
    agaff=nc.dram_tensor("ag_aff",[16384,16],F32); io["ag_aff"]=agaff.ap()
    yeo=nc.dram_tensor("ye_out",[4096,2048],F32); io["ye_out"]=yeo
    idxo=nc.dram_tensor("idx_out",[4096,1],I32); io["idx_out"]=idxo
    ag3=nc.dram_tensor("ag3",[32768,2048],F32); io["ag3"]=ag3.ap()
    agidx=nc.dram_tensor("ag_idx",[32768,1],I32); io["ag_idx"]=agidx.ap()
    io["out"]=nc.dram_tensor("out",[2048,2048],F32,kind="ExternalOutput")
    fw=FW(nc)
    rg=[list(range(8))]
    build_phase_a(nc,fw,io)
    keys=[("hg_out_m",n) for n in range(64)]+[("hg_out_h",n) for n in range(64)]
    fw.op("pool",lambda e:e.collective_compute("AllGather",ALU.bypass,replica_groups=rg,ins=[hg.ap().opt()],outs=[ag1.ap().opt()]),keys,["ag1"],kind="cc")
    fw.emit()
    build_phase_b(nc,fw,io)
    fw.op("pool",lambda e:e.collective_compute("AllGather",ALU.bypass,replica_groups=rg,ins=[h1b.ap().opt()],outs=[ag2.ap().opt()]),[("h1b_out",t) for t in range(16)],["ag2"],kind="cc")
    fw.op("pool",lambda e:e.collective_compute("AllGather",ALU.bypass,replica_groups=rg,ins=[affo.ap().opt()],outs=[agaff.ap().opt()]),[("aff_out",t) for t in range(16)],["ag_aff"],kind="cc")
    fw.emit()
    build_phase_c(nc,fw,io)
    yk=[("ye_out",L,jb,dmb) for L in range(4) for jb in range(8) for dmb in range(8)]
    fw.op("pool",lambda e:e.collective_compute("AllGather",ALU.bypass,replica_groups=rg,ins=[yeo.ap().opt()],outs=[ag3.ap().opt()]),yk,["ag3"],kind="cc")
    fw.op("pool",lambda e:e.collective_compute("AllGather",ALU.bypass,replica_groups=rg,ins=[idxo.ap().opt()],outs=[agidx.ap().opt()]),[("idx_out",L) for L in range(4)],["ag_idx"],kind="cc")
    fw.emit()
    build_phase_d(nc,fw,io)
    fw.final_wait()
    fw.close()
    return nc

def make_maps(inp):
    consts=host_consts(); consts.update(host_consts_c())
    maps=[]
    for c in range(8):
        d=prep_A(inp,c,consts); d.update(prep_B(inp,c)); d.update(prep_CD(inp,c)); maps.append(d)
    return maps


_NC = None


def kernel(**inputs):
    global _NC
    inp = {k: np.asarray(v) for k, v in inputs.items()}
    if _NC is None:
        _NC = build_full()
    maps = make_maps(inp)
    res = run_bass_kernel_spmd(_NC, maps, core_ids=list(range(8)))
    out = np.stack([np.asarray(r["out"]) for r in res.results]).reshape(2, 8192, 2048)
    return out.astype(np.float32)
```
